# Optimizing a Trainium2 kernel written in Bass

```python
import math
import numpy as np
import jax
import jax.numpy as jnp
from jax import lax

D_MODEL = 1024
BATCH = 16
SEQ = 2048
DEPTH = 1

NSA_HEADS = 8
NSA_KV_GROUPS = 2
NSA_REP = NSA_HEADS // NSA_KV_GROUPS
NSA_HD = 64
CMP_LEN = 32
CMP_STRIDE = 16
CMP_HIDDEN = 2 * NSA_HD
SEL_BLOCK = 64
SEL_TOPN = 8
SEL_FORCED_LOCAL = 2
WINDOW = 512
DIFF_HEADS = 8
DIFF_HD = 32
D_MIX = NSA_HEADS * NSA_HD + DIFF_HEADS * 2 * DIFF_HD
REL_BUCKETS = 32
REL_MAX_EXACT = 16
REL_MAX_DIST = 128
N_REL_HEADS = NSA_HEADS + DIFF_HEADS
Q_BLOCK = 128
SEL_Q_BLOCK = 64
MOE_GROUPS = 4
EXPERTS_PER_GROUP = 8
N_EXPERTS = MOE_GROUPS * EXPERTS_PER_GROUP
EXPERT_FF = 256
MOE_TOPK = 2
EPS = 1e-6
NEG = -1e30
NSA_Q = NSA_HEADS * NSA_HD
NSA_KV = NSA_KV_GROUPS * NSA_HD
NSA_GATE = NSA_HEADS * 3
DIFF_QK = DIFF_HEADS * 2 * DIFF_HD
DIFF_V = DIFF_HEADS * 2 * DIFF_HD
IN_SIZES = (NSA_Q, NSA_KV, NSA_KV, NSA_KV, NSA_KV, NSA_KV, NSA_KV, NSA_GATE, DIFF_QK, DIFF_QK, DIFF_V)
D_IN = sum(IN_SIZES)

kernel_name = 'hybrid_nsa_diffattn_hiermoe'


def rmsnorm(x, g):
    xf = x.astype(jnp.float32)
    y = xf * lax.rsqrt(jnp.mean(xf * xf, axis=-1, keepdims=True) + EPS)
    return (y * g.astype(jnp.float32)).astype(x.dtype)


def t5_bucket(dist):
    n = jnp.maximum(dist, 0)
    nf = jnp.maximum(n, 1).astype(jnp.float32)
    large = REL_MAX_EXACT + (jnp.log(nf / REL_MAX_EXACT) / math.log(REL_MAX_DIST / REL_MAX_EXACT)
                             * (REL_BUCKETS - REL_MAX_EXACT)).astype(jnp.int32)
    large = jnp.minimum(large, REL_BUCKETS - 1)
    return jnp.where(n < REL_MAX_EXACT, n, large)


def masked_softmax(s, mask):
    s = jnp.where(mask, s.astype(jnp.float32), NEG)
    p = jax.nn.softmax(s, axis=-1)
    return jnp.where(mask, p, 0.0)


def merge_blocks(out, axis):
    out = jnp.moveaxis(out, 0, axis)
    shp = out.shape
    return out.reshape(shp[:axis] + (shp[axis] * shp[axis + 1],) + shp[axis + 2:])


def nsa_mixer(q, kc, vc, ks, vs, kw, vw, gate_logits, tab_a, pos_k, pos_v, ck1, ck2, cv1, cv2):
    B, S, _ = q.shape
    G, R, hd = NSA_KV_GROUPS, NSA_REP, NSA_HD
    scale = hd ** -0.5
    tpos = jnp.arange(S, dtype=jnp.int32)
    tab = tab_a.reshape(REL_BUCKETS, G, R)
    q = q.reshape(B, S, G, R, hd).transpose(0, 2, 3, 1, 4)

    def kv_heads(t):
        return t.reshape(B, S, G, hd).transpose(0, 2, 1, 3)
    kc, vc, ks, vs, kw, vw = (kv_heads(t) for t in (kc, vc, ks, vs, kw, vw))

    n_cmp = (S - CMP_LEN) // CMP_STRIDE + 1
    starts = np.arange(n_cmp) * CMP_STRIDE
    blk_idx = starts[:, None] + np.arange(CMP_LEN)[None, :]

    def compress(t, pos, w1, w2):
        blocks = (t[:, :, blk_idx] + pos).reshape(B, G, n_cmp, CMP_LEN * hd)
        return jax.nn.gelu(blocks @ w1) @ w2
    k_cmp = compress(kc, pos_k, ck1, ck2)
    v_cmp = compress(vc, pos_v, cv1, cv2)
    cmp_end = jnp.asarray(starts + CMP_LEN - 1, dtype=jnp.int32)
    dist_c = tpos[:, None] - cmp_end[None, :]
    s_c = jnp.einsum('bgrtd,bgcd->bgrtc', q, k_cmp) * scale + tab[t5_bucket(dist_c)].transpose(2, 3, 0, 1)
    p_cmp = masked_softmax(s_c, dist_c >= 0)
    o_cmp = jnp.einsum('bgrtc,bgcd->bgrtd', p_cmp.astype(v_cmp.dtype), v_cmp)

    n_sel = S // SEL_BLOCK
    sel_start = np.arange(n_sel) * SEL_BLOCK
    overlap = ((starts[:, None] <= sel_start[None, :] + SEL_BLOCK - 1)
               & (starts[:, None] + CMP_LEN - 1 >= sel_start[None, :])).astype(np.float32)
    imp = jnp.einsum('bgrtc,cj->bgtj', p_cmp, jnp.asarray(overlap))
    blk = jnp.arange(n_sel, dtype=jnp.int32)[None, :]
    cur = (tpos // SEL_BLOCK)[:, None]
    valid = blk <= cur
    forced = (blk == 0) | ((cur - blk >= 0) & (cur - blk < SEL_FORCED_LOCAL))
    score = jnp.where(valid & forced, 1e9, jnp.where(valid, imp, -1e9))
    n_top = min(SEL_TOPN, n_sel)
    top_val, top_idx = lax.top_k(score, n_top)
    top_ok = top_val > -1e8

    ks_blk = ks.reshape(B, G, n_sel, SEL_BLOCK, hd)
    vs_blk = vs.reshape(B, G, n_sel, SEL_BLOCK, hd)
    bi = jnp.arange(B)[:, None, None, None]
    gi = jnp.arange(G)[None, :, None, None]
    tab_g = tab.transpose(1, 0, 2)

    def sel_block(c):
        t0 = c * SEL_Q_BLOCK
        qc = lax.dynamic_slice_in_dim(q, t0, SEL_Q_BLOCK, axis=3)
        ic = lax.dynamic_slice_in_dim(top_idx, t0, SEL_Q_BLOCK, axis=2)
        okc = lax.dynamic_slice_in_dim(top_ok, t0, SEL_Q_BLOCK, axis=2)
        kg = ks_blk[bi, gi, ic].reshape(B, G, SEL_Q_BLOCK, n_top * SEL_BLOCK, hd)
        vg = vs_blk[bi, gi, ic].reshape(B, G, SEL_Q_BLOCK, n_top * SEL_BLOCK, hd)
        kpos = (ic[..., None] * SEL_BLOCK + jnp.arange(SEL_BLOCK, dtype=jnp.int32)).reshape(
            B, G, SEL_Q_BLOCK, n_top * SEL_BLOCK)
        tq = t0 + jnp.arange(SEL_Q_BLOCK, dtype=jnp.int32)
        dist = tq[None, None, :, None] - kpos
        mask = jnp.repeat(okc, SEL_BLOCK, axis=-1) & (dist >= 0)
        bias = jnp.moveaxis(tab_g[gi, t5_bucket(dist)], -1, 2)
        s = jnp.einsum('bgrqd,bgqkd->bgrqk', qc, kg) * scale + bias
        p = masked_softmax(s, mask[:, :, None])
        return jnp.einsum('bgrqk,bgqkd->bgrqd', p.astype(vg.dtype), vg)
    o_slc = merge_blocks(lax.map(sel_block, jnp.arange(S // SEL_Q_BLOCK)), 3)

    pad = ((0, 0), (0, 0), (WINDOW, 0), (0, 0))
    kw_pad = jnp.pad(kw, pad)
    vw_pad = jnp.pad(vw, pad)
    span = Q_BLOCK + WINDOW

    def win_block(c):
        t0 = c * Q_BLOCK
        qc = lax.dynamic_slice_in_dim(q, t0, Q_BLOCK, axis=3)
        kb = lax.dynamic_slice_in_dim(kw_pad, t0, span, axis=2)
        vb = lax.dynamic_slice_in_dim(vw_pad, t0, span, axis=2)
        tq = t0 + jnp.arange(Q_BLOCK, dtype=jnp.int32)
        kpos = t0 - WINDOW + jnp.arange(span, dtype=jnp.int32)
        dist = tq[:, None] - kpos[None, :]
        mask = (kpos[None, :] >= 0) & (dist >= 0) & (dist < WINDOW)
        bias = tab[t5_bucket(dist)].transpose(2, 3, 0, 1)
        s = jnp.einsum('bgrqd,bgkd->bgrqk', qc, kb) * scale + bias
        p = masked_softmax(s, mask)
        return jnp.einsum('bgrqk,bgkd->bgrqd', p.astype(vb.dtype), vb)
    o_win = merge_blocks(lax.map(win_block, jnp.arange(S // Q_BLOCK)), 3)

    g = jax.nn.sigmoid(gate_logits.astype(jnp.float32)).astype(o_win.dtype)
    g = g.reshape(B, S, G, R, 3).transpose(0, 2, 3, 1, 4)
    o = g[..., 0:1] * o_cmp + g[..., 1:2] * o_slc + g[..., 2:3] * o_win
    return o.transpose(0, 3, 1, 2, 4).reshape(B, S, NSA_HEADS * hd)


def diff_mixer(q, k, v, tab_b, lq1, lk1, lq2, lk2, subln, lambda_init):
    B, S, _ = q.shape
    H, d = DIFF_HEADS, DIFF_HD
    scale = d ** -0.5
    q = q.reshape(B, S, H, 2, d).transpose(3, 0, 2, 1, 4)
    k = k.reshape(B, S, H, 2, d).transpose(3, 0, 2, 1, 4)
    v = v.reshape(B, S, H, 2 * d).transpose(0, 2, 1, 3)
    lam = (jnp.exp(jnp.sum((lq1 * lk1).astype(jnp.float32)))
           - jnp.exp(jnp.sum((lq2 * lk2).astype(jnp.float32))) + lambda_init)
    kpos = jnp.arange(S, dtype=jnp.int32)

    def blk(c):
        t0 = c * Q_BLOCK
        qc = lax.dynamic_slice_in_dim(q, t0, Q_BLOCK, axis=3)
        tq = t0 + jnp.arange(Q_BLOCK, dtype=jnp.int32)
        dist = tq[:, None] - kpos[None, :]
        bias = tab_b[t5_bucket(dist)].transpose(2, 0, 1)
        s = jnp.einsum('ibhqd,ibhkd->ibhqk', qc, k) * scale + bias
        p = masked_softmax(s, dist >= 0)
        a = p[0] - lam * p[1]
        return jnp.einsum('bhqk,bhkd->bhqd', a.astype(v.dtype), v)
    o = merge_blocks(lax.map(blk, jnp.arange(S // Q_BLOCK)), 2)
    o = rmsnorm(o, subln) * (1.0 - lambda_init)
    return o.transpose(0, 2, 1, 3).reshape(B, S, H * 2 * d)


def hier_moe(h, wg, bg, we, be, w_gate, w_up, w_down):
    B, S, D = h.shape
    t = h.reshape(B * S, D)
    T = t.shape[0]
    pg = jax.nn.softmax((t @ wg).astype(jnp.float32) + bg.astype(jnp.float32), axis=-1)
    g_prob, g_idx = lax.top_k(pg, 1)
    e_logit = ((t @ we).astype(jnp.float32) + be.astype(jnp.float32)).reshape(T, MOE_GROUPS, EXPERTS_PER_GROUP)
    e_in = jnp.take_along_axis(e_logit, g_idx[:, :, None], axis=1)[:, 0]
    pe = jax.nn.softmax(e_in, axis=-1)
    e_prob, e_idx = lax.top_k(pe, MOE_TOPK)
    w = g_prob * e_prob / jnp.sum(e_prob, axis=-1, keepdims=True)
    expert_id = g_idx * EXPERTS_PER_GROUP + e_idx
    combine = jnp.sum(jax.nn.one_hot(expert_id, N_EXPERTS, dtype=jnp.float32) * w[..., None], axis=1)
    combine = combine.astype(h.dtype)
    y = jnp.zeros_like(t)
    for gidx in range(MOE_GROUPS):
        sl = slice(gidx * EXPERTS_PER_GROUP, (gidx + 1) * EXPERTS_PER_GROUP)
        hg = jax.nn.silu(jnp.einsum('td,edf->tef', t, w_gate[sl])) * jnp.einsum('td,edf->tef', t, w_up[sl])
        y = y + jnp.einsum('tef,efd->td', hg * combine[:, sl, None], w_down[sl])
    return y.reshape(B, S, D)


def setup_inputs(seed: int = 0) -> dict:
    key = jax.random.key(seed)
    ks = jax.random.split(key, 32)

    def nrm(k, shape, scale):
        return jax.random.normal(k, shape, jnp.float32) * scale

    def gain(k, shape):
        return 1.0 + 0.01 * jax.random.normal(k, shape, jnp.float32)
    return {
        'x': nrm(ks[0], (BATCH, SEQ, D_MODEL), 1.0),
        'rel_bias': nrm(ks[1], (REL_BUCKETS, N_REL_HEADS), 0.5),
        'ln_mix': gain(ks[2], (DEPTH, D_MODEL)),
        'w_in': nrm(ks[3], (DEPTH, D_MODEL, D_IN), D_MODEL ** -0.5),
        'cmp_pos_k': nrm(ks[4], (DEPTH, CMP_LEN, NSA_HD), 0.1),
        'cmp_pos_v': nrm(ks[5], (DEPTH, CMP_LEN, NSA_HD), 0.1),
        'cmp_k_w1': nrm(ks[6], (DEPTH, CMP_LEN * NSA_HD, CMP_HIDDEN), (CMP_LEN * NSA_HD) ** -0.5),
        'cmp_k_w2': nrm(ks[7], (DEPTH, CMP_HIDDEN, NSA_HD), CMP_HIDDEN ** -0.5),
        'cmp_v_w1': nrm(ks[8], (DEPTH, CMP_LEN * NSA_HD, CMP_HIDDEN), (CMP_LEN * NSA_HD) ** -0.5),
        'cmp_v_w2': nrm(ks[9], (DEPTH, CMP_HIDDEN, NSA_HD), CMP_HIDDEN ** -0.5),
        'diff_lq1': nrm(ks[10], (DEPTH, DIFF_HD), 0.1),
        'diff_lk1': nrm(ks[11], (DEPTH, DIFF_HD), 0.1),
        'diff_lq2': nrm(ks[12], (DEPTH, DIFF_HD), 0.1),
        'diff_lk2': nrm(ks[13], (DEPTH, DIFF_HD), 0.1),
        'diff_subln': gain(ks[14], (DEPTH, 2 * DIFF_HD)),
        'w_out': nrm(ks[15], (DEPTH, D_MIX, D_MODEL), D_MIX ** -0.5),
        'ln_ffn': gain(ks[16], (DEPTH, D_MODEL)),
        'router_group_w': nrm(ks[17], (DEPTH, D_MODEL, MOE_GROUPS), D_MODEL ** -0.5),
        'router_group_b': nrm(ks[18], (DEPTH, MOE_GROUPS), 0.01),
        'router_expert_w': nrm(ks[19], (DEPTH, D_MODEL, N_EXPERTS), D_MODEL ** -0.5),
        'router_expert_b': nrm(ks[20], (DEPTH, N_EXPERTS), 0.01),
        'exp_w_gate': nrm(ks[21], (DEPTH, N_EXPERTS, D_MODEL, EXPERT_FF), D_MODEL ** -0.5),
        'exp_w_up': nrm(ks[22], (DEPTH, N_EXPERTS, D_MODEL, EXPERT_FF), D_MODEL ** -0.5),
        'exp_w_down': nrm(ks[23], (DEPTH, N_EXPERTS, EXPERT_FF, D_MODEL), EXPERT_FF ** -0.5),
        'ln_final': gain(ks[24], (D_MODEL,)),
    }


def reference(x, rel_bias, ln_mix, w_in, cmp_pos_k, cmp_pos_v, cmp_k_w1, cmp_k_w2, cmp_v_w1, cmp_v_w2,
              diff_lq1, diff_lk1, diff_lq2, diff_lk2, diff_subln, w_out, ln_ffn,
              router_group_w, router_group_b, router_expert_w, router_expert_b,
              exp_w_gate, exp_w_up, exp_w_down, ln_final):
    split_points = [int(v) for v in np.cumsum(IN_SIZES)[:-1]]
    tab_a = rel_bias[:, :NSA_HEADS]
    tab_b = rel_bias[:, NSA_HEADS:]
    h = x
    for l in range(DEPTH):
        u = rmsnorm(h, ln_mix[l]) @ w_in[l]
        q_a, kc, vc, ks, vs, kw, vw, gates, q_b, k_b, v_b = jnp.split(u, split_points, axis=-1)
        o_a = nsa_mixer(q_a, kc, vc, ks, vs, kw, vw, gates, tab_a, cmp_pos_k[l], cmp_pos_v[l],
                        cmp_k_w1[l], cmp_k_w2[l], cmp_v_w1[l], cmp_v_w2[l])
        lambda_init = 0.8 - 0.6 * math.exp(-0.3 * l)
        o_b = diff_mixer(q_b, k_b, v_b, tab_b, diff_lq1[l], diff_lk1[l], diff_lq2[l], diff_lk2[l],
                         diff_subln[l], lambda_init)
        h = h + jnp.concatenate([o_a, o_b], axis=-1) @ w_out[l]
        h = h + hier_moe(rmsnorm(h, ln_ffn[l]), router_group_w[l], router_group_b[l],
                         router_expert_w[l], router_expert_b[l], exp_w_gate[l], exp_w_up[l], exp_w_down[l])
    return rmsnorm(h, ln_final)
```

```python
import math
from contextlib import ExitStack

import numpy as np
import ml_dtypes

import concourse.bass as bass
import concourse.mybir as mybir
from concourse.bass_utils import run_bass_kernel_spmd

F32 = mybir.dt.float32
BF16 = mybir.dt.bfloat16
AF = mybir.ActivationFunctionType
ALU = mybir.AluOpType
AX = mybir.AxisListType

S = 2048
D = 1024
NT = 16
KC = 8
D_IN = 2840
NEGB = -30000.0
EPS = 1e-6
SC_A = 0.125
SC_B = 32 ** -0.5
LAMBDA_INIT = 0.8 - 0.6 * math.exp(-0.3 * 0)
TINY = 1e-30

C_QA, C_KC, C_VC, C_KS, C_VS, C_KW, C_VW, C_GT, C_QB, C_KB, C_VB = (
    0, 512, 640, 768, 896, 1024, 1152, 1280, 1304, 1816, 2328)

SELF_RAW = True
STRICT_SELF = True


def _bucket(n):
    n = np.maximum(n, 0)
    nf = np.maximum(n, 1).astype(np.float32)
    large = 16 + (np.log(nf / np.float32(16)) / np.float32(math.log(128 / 16)) * np.float32(16)).astype(np.int32)
    large = np.minimum(large, 31)
    return np.where(n < 16, n, large)


def _host_consts():
    c = {}
    c["ident"] = np.eye(128, dtype=np.float32)
    k = np.arange(128)[:, None]
    t = np.arange(128)[None, :]
    m_ = np.arange(384)[None, :] - 127
    bk1 = _bucket(m_)
    c["oh1"] = ((bk1 == np.arange(32)[:, None]) & (m_ >= 0)).astype(np.float32)
    c["mask1"] = np.broadcast_to(np.where(m_ >= 0, 0.0, NEGB).astype(np.float32), (128, 384)).copy()
    cm = np.eye(32, dtype=np.float32)
    cm[31, :] -= 1.0
    c["cmat"] = cm
    c["mask4"] = np.where(t < k, 0.0, NEGB).astype(np.float32)
    j = np.arange(247)[None, :]
    tt = np.arange(128)[:, None]
    dc = tt - 16 * (j - 120) - 31
    bkc = _bucket(dc)
    ohc = np.zeros((128, 31, 247), np.float32)
    for b in range(31):
        ohc[:, b, :] = ((bkc == b) & (dc >= 0))
    c["ohc"] = ohc.astype(ml_dtypes.bfloat16)
    c["maskc"] = np.where(dc >= 0, 0.0, NEGB).astype(np.float32)
    starts = np.arange(127) * 16
    sel_start = np.arange(32) * 64
    ovl = ((starts[:, None] <= sel_start[None, :] + 63) & (starts[:, None] + 31 >= sel_start[None, :]))
    c["ovl"] = ovl.astype(np.float32)
    tpos = (np.arange(16)[None, :, None] * 128 + np.arange(128)[:, None, None])
    cur = tpos // 64
    blk = np.arange(32)[None, None, :]
    valid = blk <= cur
    forced = (blk == 0) | ((cur - blk >= 0) & (cur - blk < 2))
    c["nfv"] = (valid & ~forced).astype(np.float32)
    c["addc"] = np.where(valid & forced, 1e9, np.where(valid, 0.0, -1e9)).astype(np.float32)
    c["valid"] = valid.astype(np.float32)
    e = (np.arange(2048)[None, :] // 64 == np.arange(32)[:, None])
    c["eexp"] = e.astype(np.float32)
    return c


class Buf:
    __slots__ = ("name", "w", "r", "dw", "dr", "excl")

    def __init__(self, name, excl=False):
        self.name = name
        self.excl = excl
        self.w = {}
        self.r = {}
        self.dw = []
        self.dr = []


class Eng:
    def __init__(self, name, h, sem):
        self.name = name
        self.h = h
        self.sem = sem
        self.cnt = 0
        self.known = {}


class K:
    def __init__(self, nc, es):
        self.nc = nc
        self.es = es
        self.eng = {}
        for name, h in (("pe", nc.tensor), ("act", nc.scalar), ("dve", nc.vector), ("pool", nc.gpsimd),
                        ("sp", nc.sync)):
            sem = es.enter_context(nc.semaphore("sem_" + name))
            self.eng[name] = Eng(name, h, sem)
        self.ndsem = 20
        self.dsem = {}
        self.dval = {}
        self.drr = {}
        for q in ("sp", "pool"):
            self.dsem[q] = [es.enter_context(nc.semaphore(f"dsem_{q}{i}")) for i in range(self.ndsem)]
            self.dval[q] = [0] * self.ndsem
            self.dbar = getattr(self, "dbar", {})
            self.dbar[q] = [0] * self.ndsem
            self.drr[q] = 0
        self.nbuf = 0
        self.out_dmas = []

    def buf(self, name=None, excl=False):
        self.nbuf += 1
        return Buf(name or f"b{self.nbuf}", excl)

    def pbuf(self, name=None):
        return self.buf(name, excl=True)

    def _semof(self, key):
        if key[0] == "e":
            return self.eng[key[1]].sem
        return self.dsem[key[1]][key[2]]

    def _collect(self, eng, reads, writes):
        waits = {}

        def need(key, val):
            if val > waits.get(key, 0):
                waits[key] = val
        for b in reads:
            for en, idx in b.w.items():
                if en == eng and (eng == "pe" or not SELF_RAW):
                    continue
                need(("e", en), idx)
            for key, v in b.dw:
                need(key, v)
            if b.excl:
                for en, idx in b.r.items():
                    if en != eng:
                        need(("e", en), idx)
        for b in writes:
            for en, idx in b.w.items():
                if en == eng and (eng == "pe" or not STRICT_SELF):
                    continue
                need(("e", en), idx)
            for en, idx in b.r.items():
                if en == eng and (eng == "pe" or not STRICT_SELF):
                    continue
                need(("e", en), idx)
            for key, v in b.dw:
                need(key, v)
            for key, v in b.dr:
                need(key, v)
        return waits

    def _emit_waits(self, E, waits):
        for key, val in waits.items():
            if E.known.get(key, 0) < val:
                E.h.wait_ge(self._semof(key), val)
                E.known[key] = val

    def op(self, eng, fn, reads=(), writes=()):
        E = self.eng[eng]
        self._emit_waits(E, self._collect(eng, reads, writes))
        ins = fn(E.h)
        E.cnt += 1
        ins.then_inc(E.sem, 1)
        for b in reads:
            b.r[eng] = E.cnt
        for b in writes:
            b.w = {eng: E.cnt}
            b.r = {}
            b.dw = []
            b.dr = []
        return E.cnt

    def dma(self, q, out_ap, in_ap, reads=(), writes=(), is_output=False, nobarrier=False):
        E = self.eng[q]
        self._emit_waits(E, self._collect(q, reads, writes))
        i = self.drr[q]
        self.drr[q] = (i + 1) % self.ndsem
        key = ("d", q, i)
        if E.known.get(key, 0) < self.dval[q][i]:
            E.h.wait_ge(self.dsem[q][i], self.dval[q][i])
            E.known[key] = self.dval[q][i]
        self.dval[q][i] += 16
        val = self.dval[q][i]
        if not nobarrier:
            self.dbar[q][i] = val
        E.h.dma_start(out=out_ap, in_=in_ap).then_inc(self.dsem[q][i], 16)
        for b in reads:
            b.dr.append((key, val))
            if len(b.dr) > 8:
                b.dr = b.dr[-8:]
        for b in writes:
            if b.r or b.w or b.dr:
                b.dw = []
            b.dw.append((key, val))
            b.w = {}
            b.r = {}
            b.dr = []
        if is_output:
            self.out_dmas.append((key, val))

    def defer(self, eng, fn, reads=(), writes=(), tag=None):
        if not hasattr(self, "pending"):
            self.pending = []
        self.pending.append((eng, fn, list(reads), list(writes), tag))

    def drain(self, n=None, until_tag=None):
        pend = getattr(self, "pending", [])
        k = 0
        while pend:
            if until_tag is not None and not any(t[4] == until_tag for t in pend):
                break
            if until_tag is None and n is not None and k >= n:
                break
            eng, fn, r, w, _ = pend.pop(0)
            self.op(eng, fn, reads=r, writes=w)
            k += 1

    def barrier(self):
        finals = {("e", n): self.eng[n].cnt for n in ("pe", "act", "dve", "pool")}
        for q in ("sp", "pool"):
            for i in range(self.ndsem):
                finals[("d", q, i)] = self.dbar[q][i]
        for n in ("pe", "act", "dve", "pool", "sp"):
            E = self.eng[n]
            w = {k: v for k, v in finals.items() if v > 0 and not (k[0] == "e" and k[1] == n)}
            self._emit_waits(E, w)


def build_nc(nseq=2, stage=99, dbg=False, nexp=32):
    nc = bass.Bass("TRN2", target_bir_lowering=False)
    hc = _host_consts()

    def din(name, shape, dt=F32):
        return nc.dram_tensor(name, list(shape), dt, kind="ExternalInput")

    x_d = din("x", [nseq, S, D])
    relb_d = din("rel_bias", [32, 16])
    lnmix_d = din("ln_mix", [1, D])
    win_d = din("w_in", [D, D_IN])
    posk_d = din("cmp_pos_k", [32, 64])
    posv_d = din("cmp_pos_v", [32, 64])
    ck1_d = din("cmp_k_w1", [2048, 128])
    ck2_d = din("cmp_k_w2", [128, 64])
    cv1_d = din("cmp_v_w1", [2048, 128])
    cv2_d = din("cmp_v_w2", [128, 64])
    lq1_d = din("diff_lq1", [1, 32])
    lk1_d = din("diff_lk1", [1, 32])
    lq2_d = din("diff_lq2", [1, 32])
    lk2_d = din("diff_lk2", [1, 32])
    subln_d = din("diff_subln", [1, 64])
    wout_d = din("w_out", [D, D])
    lnffn_d = din("ln_ffn", [1, D])
    rgw_d = din("router_group_w", [D, 4])
    rgb_d = din("router_group_b", [1, 4])
    rew_d = din("router_expert_w", [D, 32])
    reb_d = din("router_expert_b", [1, 32])
    wg_d = din("exp_w_gate", [nexp, D, 256])
    wu_d = din("exp_w_up", [nexp, D, 256])
    wd_d = din("exp_w_down", [nexp, 256, D])
    lnfin_d = din("ln_final", [1, D])
    hcd = {}
    for k_, v_ in hc.items():
        hcd[k_] = din("hc_" + k_, v_.shape, BF16 if v_.dtype == ml_dtypes.bfloat16 else F32)
    out_d = nc.dram_tensor("out", [nseq, S, D], F32, kind="ExternalOutput")
    scr_d = nc.dram_tensor("scr_toeplitz", [16, 128, 384], F32)
    dbg_out = {}

    es = ExitStack()
    with es:
        kk = K(nc, es)
        es.enter_context(nc.Block())

        uid = [0]

        def sb(name, shape, dt, stack=es):
            uid[0] += 1
            return stack.enter_context(nc.sbuf_tensor(f"{name}_{uid[0]}", list(shape), dt))

        def psum(name, shape, dt, stack=es):
            uid[0] += 1
            return stack.enter_context(nc.psum_tensor(f"{name}_{uid[0]}", list(shape), dt))

        def bcast_rows(dt_, n):
            return bass.AP(dt_, 0, [[0, 128], [1, n]])

        def dump(name, ap_sb, shape, bufs, dt=F32):
            if not dbg:
                return
            t = nc.dram_tensor("dbg_" + name, list(shape), dt, kind="ExternalOutput")
            dbg_out[name] = t
            kk.dma("sp", t.ap(), ap_sb, reads=bufs, is_output=True)

        ident_f = sb("ident_f", [128, 128], F32)
        ident_b = sb("ident_b", [128, 128], BF16)
        B_const = kk.buf("const")
        kk.dma("sp", ident_f[:], hcd["ident"].ap(), writes=[B_const])
        kk.dma("pool", ident_b[:], hcd["ident"].ap(), writes=[B_const])
        mask4 = sb("mask4", [128, 128], F32)
        kk.dma("sp", mask4[:], hcd["mask4"].ap(), writes=[B_const])
        ovl = sb("ovl", [127, 32], F32)
        kk.dma("sp", ovl[:], hcd["ovl"].ap(), writes=[B_const])
        subln = sb("subln", [128, 64], F32)
        kk.dma("sp", subln[:], bcast_rows(subln_d, 64), writes=[B_const])
        rbias = sb("rbias", [128, 36], F32)
        kk.dma("sp", rbias[:, 0:4], bcast_rows(rgb_d, 4), writes=[B_const])
        kk.dma("sp", rbias[:, 4:36], bcast_rows(reb_d, 32), writes=[B_const])
        wr = sb("wr", [128, KC, 36], F32)
        kk.dma("sp", wr[:, :, 0:4], rgw_d.ap().rearrange("(kc p) c -> p kc c", p=128), writes=[B_const])
        kk.dma("sp", wr[:, :, 4:36], rew_d.ap().rearrange("(kc p) c -> p kc c", p=128), writes=[B_const])
        w2k = sb("w2k", [128, 64], BF16)
        w2v = sb("w2v", [128, 64], BF16)
        B_cw = kk.buf("cw")
        kk.dma("pool", w2k[:], ck2_d.ap(), writes=[B_cw])
        kk.dma("pool", w2v[:], cv2_d.ap(), writes=[B_cw])

        W1 = sb("W1", [128, KC, 1536], BF16)
        B_W1 = kk.buf("W1")
        win_v = win_d.ap().rearrange("(kc p) c -> p kc c", p=128)
        wout_v = wout_d.ap().rearrange("(kc p) c -> p kc c", p=128)

        def load_W1(which):
            if which == "A":
                for c0 in range(0, 1304, 326):
                    kk.dma("pool", W1[:, :, c0:c0 + 326], win_v[:, :, c0:c0 + 326], writes=[B_W1], nobarrier=True)
            elif which == "B":
                for c0 in range(0, 1536, 384):
                    kk.dma("pool", W1[:, :, c0:c0 + 384], win_v[:, :, C_QB + c0:C_QB + c0 + 384], writes=[B_W1], nobarrier=True)
            else:
                for c0 in range(0, D, 256):
                    kk.dma("pool", W1[:, :, c0:c0 + 256], wout_v[:, :, c0:c0 + 256], writes=[B_W1], nobarrier=True)

        lqk = sb("lqk", [128, 4, 32], F32)
        B_l = kk.buf("lam")
        for i_, t_ in enumerate((lq1_d, lk1_d, lq2_d, lk2_d)):
            kk.dma("sp", lqk[:, i_, :], bcast_rows(t_, 32), writes=[B_l])
        lam_t = sb("lam_t", [128, 8], F32)
        neglam = sb("neglam", [128, 1], F32)
        kk.op("dve", lambda e: e.tensor_tensor(out=lqk[:, 0, :], in0=lqk[:, 0, :], in1=lqk[:, 1, :], op=ALU.mult),
              reads=[B_l], writes=[B_l])
        kk.op("dve", lambda e: e.tensor_tensor(out=lqk[:, 2, :], in0=lqk[:, 2, :], in1=lqk[:, 3, :], op=ALU.mult),
              reads=[B_l], writes=[B_l])
        kk.op("dve", lambda e: e.reduce_sum(out=lam_t[:, 0:1], in_=lqk[:, 0, :], axis=AX.X), reads=[B_l], writes=[B_l])
        kk.op("dve", lambda e: e.reduce_sum(out=lam_t[:, 1:2], in_=lqk[:, 2, :], axis=AX.X), reads=[B_l], writes=[B_l])
        kk.op("act", lambda e: e.activation(out=lam_t[:, 2:4], in_=lam_t[:, 0:2], func=AF.Exp), reads=[B_l], writes=[B_l])
        kk.op("dve", lambda e: e.tensor_tensor(out=lam_t[:, 4:5], in0=lam_t[:, 3:4], in1=lam_t[:, 2:3], op=ALU.subtract),
              reads=[B_l], writes=[B_l])
        kk.op("dve", lambda e: e.tensor_scalar_add(out=neglam[:], in0=lam_t[:, 4:5], scalar1=-LAMBDA_INIT),
              reads=[B_l], writes=[B_l])

        TN = sb("TN", [128, 2, 2, 4, 128], F32)
        TD = sb("TD", [128, 8, 256], F32)
        TC = sb("TC", [128, 2, 4, 247], F32)
        B_TN = kk.buf("TN")
        B_TD = kk.buf("TD")
        B_TC = kk.buf("TC")
        with ExitStack() as ts:
            tab = sb("tab", [128, 32, 16], F32, ts)
            val = sb("val", [128, 32, 16], F32, ts)
            ohc = sb("ohc", [128, 31, 247], BF16, ts)
            maskc = sb("maskc", [128, 247], F32, ts)
            B_t = kk.buf("tab")
            B_oh = kk.buf("oh")
            kk.dma("sp", tab[:].rearrange("p b h -> p (b h)"), bass.AP(relb_d, 0, [[0, 128], [1, 512]]), writes=[B_t])
            kk.dma("sp", ohc[:], hcd["ohc"].ap(), writes=[B_oh])
            kk.dma("sp", maskc[:], hcd["maskc"].ap(), writes=[B_oh])
            kk.op("dve", lambda e: e.tensor_tensor(out=val[:], in0=tab[:], in1=tab[:, 31:32, :].to_broadcast([128, 32, 16]),
                                                   op=ALU.subtract), reads=[B_t], writes=[B_t])
            kk.op("dve", lambda e: e.tensor_scalar_mul(out=val[:, :, 0:8], in0=val[:, :, 0:8], scalar1=1.0 / SC_A),
                  reads=[B_t], writes=[B_t])
            kk.op("dve", lambda e: e.tensor_scalar_mul(out=val[:, :, 8:16], in0=val[:, :, 8:16], scalar1=1.0 / SC_B),
                  reads=[B_t], writes=[B_t])
            cmat = sb("cmat", [32, 32], F32, ts)
            oh1 = sb("oh1", [32, 384], F32, ts)
            mask1 = sb("mask1", [128, 384], F32, ts)
            tab32 = sb("tab32", [32, 16], F32, ts)
            vs32 = sb("vs32", [32, 16], F32, ts)
            valb = sb("valb", [32, 16, 128], F32, ts)
            frep = sb("frep", [128, 16, 384], F32, ts)
            pv32 = psum("pv32", [128, 512], F32, ts)
            pF = [psum(f"pF{i}", [128, 512], F32, ts) for i in range(2)]
            B_sk = kk.buf()
            B_pv = kk.pbuf()
            B_pF = [kk.pbuf() for _ in range(2)]
            B_fr = kk.buf()
            kk.dma("sp", cmat[:], hcd["cmat"].ap(), writes=[B_sk])
            kk.dma("sp", oh1[:], hcd["oh1"].ap(), writes=[B_sk])
            kk.dma("sp", mask1[:], hcd["mask1"].ap(), writes=[B_sk])
            kk.dma("sp", tab32[:], relb_d.ap(), writes=[B_sk])
            kk.op("pe", lambda e: e.matmul(pv32[0:32, 0:16], lhsT=cmat[:], rhs=tab32[:], start=True, stop=True),
                  reads=[B_sk], writes=[B_pv])
            kk.op("dve", lambda e: e.tensor_scalar_mul(out=vs32[:, 0:8], in0=pv32[0:32, 0:8], scalar1=1.0 / SC_A),
                  reads=[B_pv], writes=[B_sk])
            kk.op("dve", lambda e: e.tensor_scalar_mul(out=vs32[:, 8:16], in0=pv32[0:32, 8:16], scalar1=1.0 / SC_B),
                  reads=[B_pv], writes=[B_sk])
            kk.op("dve", lambda e: e.tensor_copy(out=valb[:], in_=vs32[:].unsqueeze(2).to_broadcast([32, 16, 128])),
                  reads=[B_sk], writes=[B_sk])
            for h in range(16):
                p = h % 2
                kk.op("pe", lambda e, p=p, h=h: e.matmul(pF[p][:, 0:384], lhsT=valb[:, h, :], rhs=oh1[:], start=True, stop=True),
                      reads=[B_sk], writes=[B_pF[p]])
                kk.op("dve", lambda e, p=p, h=h: e.tensor_tensor(out=frep[:, h, :], in0=pF[p][:, 0:384], in1=mask1[:], op=ALU.add),
                      reads=[B_pF[p], B_sk], writes=[B_fr])
            B_scr = kk.buf()
            kk.dma("sp", scr_d.ap().rearrange("h p m -> p h m"), frep[:], reads=[B_fr], writes=[B_scr])
            for g in range(2):
                for r in range(4):
                    hh = 4 * g + r
                    kk.dma("sp", TN[:, g, :, r, :], bass.AP(scr_d, hh * 128 * 384 + 127, [[383, 128], [128, 2], [1, 128]]),
                           reads=[B_scr], writes=[B_TN])
            for h in range(8):
                kk.dma("sp", TD[:, h, :], bass.AP(scr_d, (8 + h) * 128 * 384 + 127, [[383, 128], [1, 256]]),
                       reads=[B_scr], writes=[B_TD])
            accs = []
            for g in range(2):
                for r in range(4):
                    bC = kk.buf()
                    engc = "dve"
                    kk.op(engc, lambda e, g=g, r=r: e.tensor_copy(out=TC[:, g, r, :], in_=maskc[:]),
                          reads=[B_oh], writes=[bC])
                    accs.append((engc, TC[:, g, r, :], lambda b: ohc[:, b, :], 4 * g + r, bC))
            ptmps = [sb(f"ptmp{i}", [128, 256], F32, ts) for i in range(4)]
            B_ptmps = [kk.buf() for _ in range(4)]
            pi = 0
            for b in range(31):
                for (eng, oap, ohf, col, bb) in accs:
                    if eng == "dve":
                        kk.op("dve", lambda e, oap=oap, ohf=ohf, col=col, b=b: e.scalar_tensor_tensor(
                            out=oap, in0=ohf(b), scalar=val[:, b, col:col + 1], in1=oap, op0=ALU.mult, op1=ALU.add),
                            reads=[B_oh, B_t, bb], writes=[bb])
                    else:
                        q_ = pi % 4
                        pi += 1
                        kk.op("pool", lambda e, ohf=ohf, col=col, b=b, q_=q_: e.tensor_scalar_mul(
                            out=ptmps[q_][:, 0:247], in0=ohf(b), scalar1=val[:, b, col:col + 1]),
                            reads=[B_oh, B_t], writes=[B_ptmps[q_]])
                        kk.op("pool", lambda e, oap=oap, q_=q_: e.tensor_tensor(
                            out=oap, in0=oap, in1=ptmps[q_][:, 0:247], op=ALU.add),
                            reads=[B_ptmps[q_], bb], writes=[bb])
            kk.barrier()
        if stage == 0:
            dump("TN", TN[:], [128, 2, 2, 4, 128], [B_TN])
            dump("TD", TD[:], [128, 8, 256], [B_TD])
            dump("TC", TC[:], [128, 2, 4, 247], [B_TC])
            dump("neglam", neglam[:], [128, 1], [B_l])

        if stage > 0:
            load_W1("A")
        for sq in range(nseq if stage > 0 else 0):
            with ExitStack() as ss:
                xT = sb("xT", [128, KC, S], BF16, ss)
                mx = ss.enter_context(ExitStack())
                omix = sb("omix", [128, NT, D], BF16, mx)
                gates = sb("gates", [128, NT, 24], F32, mx)
                B_xT = [kk.buf(f"xT{i}") for i in range(NT)]
                B_om = [kk.buf(f"om{i}") for i in range(NT)]
                B_gates = kk.buf("gates")

                with ExitStack() as ts:
                    gmix = sb("gmix", [128, D], F32, ts)
                    B_gm = kk.buf()
                    kk.dma("sp", gmix[:], bcast_rows(lnmix_d, D), writes=[B_gm])
                    NB0 = 3
                    xs = [sb(f"xs{i}", [128, D], F32, ts) for i in range(NB0)]
                    xn = [sb(f"xn{i}", [128, D], BF16, ts) for i in range(NB0)]
                    junk = sb("junkA", [128, D], BF16, ts)
                    st = sb("stA", [128, NT, 4], F32, ts)
                    ptr = [psum(f"ptrA{i}", [128, KC, 128], BF16, ts) for i in range(2)]
                    B_xs = [kk.buf() for _ in range(NB0)]
                    B_xn = [kk.buf() for _ in range(NB0)]
                    B_ptr = [kk.pbuf() for _ in range(2)]
                    B_st = [kk.buf() for _ in range(NT)]
                    B_junk = kk.buf()

                    def a0_s1(i):
                        p = i % NB0
                        kk.dma("sp", xs[p][:], x_d.ap()[sq, i * 128:(i + 1) * 128, :], writes=[B_xs[p]])
                        kk.op("act", lambda e: e.activation(out=junk[:], in_=xs[p][:], func=AF.Square,
                                                            accum_out=st[:, i, 0:1]),
                              reads=[B_xs[p]], writes=[B_st[i], B_junk])
                        kk.op("act", lambda e: e.activation(out=st[:, i, 1:2], in_=st[:, i, 0:1], func=AF.Sqrt,
                                                            bias=EPS, scale=1.0 / D), reads=[B_st[i]], writes=[B_st[i]])
                        kk.op("dve", lambda e: e.reciprocal(out=st[:, i, 2:3], in_=st[:, i, 1:2]),
                              reads=[B_st[i]], writes=[B_st[i]])
                        kk.op("dve", lambda e: e.scalar_tensor_tensor(
                            out=xn[p][:], in0=xs[p][:], scalar=st[:, i, 2:3], in1=gmix[:], op0=ALU.mult, op1=ALU.mult),
                            reads=[B_xs[p], B_st[i], B_gm], writes=[B_xn[p]])

                    def a0_s2(i):
                        p = i % NB0
                        pp_ = i % 2
                        for c in range(KC):
                            kk.op("pe", lambda e, c=c: e.transpose(ptr[pp_][:, c, :], xn[p][:, c * 128:(c + 1) * 128],
                                                                   ident_b[:]),
                                  reads=[B_xn[p], B_const], writes=[B_ptr[pp_]])
                        if i % 2 == 0:
                            kk.op("act", lambda e: e.copy(out=xT[:, :, i * 128:(i + 1) * 128], in_=ptr[pp_][:]),
                                  reads=[B_ptr[pp_]], writes=[B_xT[i]])
                        else:
                            kk.op("dve", lambda e: e.tensor_copy(out=xT[:, :, i * 128:(i + 1) * 128], in_=ptr[pp_][:]),
                                  reads=[B_ptr[pp_]], writes=[B_xT[i]])

                    for k in range(NT + 1):
                        if k < NT:
                            a0_s1(k)
                        if k >= 1:
                            a0_s2(k - 1)
                    kk.barrier()
                if stage == 1:
                    dump("xT", xT[:], [128, KC, S], B_xT, BF16)
                    continue

                with ExitStack() as ns:
                    build_nsa(nc, kk, ns, sb, psum, sq, locals(), stage, dump)
                    kk.barrier()
                if stage <= 4:
                    dump("omixA", omix[:, :, 0:512], [128, NT, 512], B_om, BF16)
                    continue
                with ExitStack() as ds:
                    build_diff(nc, kk, ds, sb, psum, sq, locals(), stage, dump)
                    kk.barrier()
                if stage == 5:
                    dump("omix", omix[:], [128, NT, D], B_om, BF16)
                    continue
                with ExitStack() as ts:
                    ptrO = [psum(f"ptrO{i}", [128, KC, 128], BF16, ts) for i in range(2)]
                    B_ptrO = [kk.pbuf() for _ in range(2)]
                    for i in range(NT):
                        p = i % 2
                        for c in range(KC):
                            kk.op("pe", lambda e, p=p, c=c, i=i: e.transpose(ptrO[p][:, c, :], omix[:, i, c * 128:(c + 1) * 128], ident_b[:]),
                                  reads=[B_om[i], B_const], writes=[B_ptrO[p]])
                        if i % 2 == 0:
                            kk.op("act", lambda e, p=p, i=i: e.copy(out=xT[:, :, i * 128:(i + 1) * 128], in_=ptrO[p][:]),
                                  reads=[B_ptrO[p]], writes=[B_xT[i]])
                        else:
                            kk.op("dve", lambda e, p=p, i=i: e.tensor_copy(out=xT[:, :, i * 128:(i + 1) * 128], in_=ptrO[p][:]),
                                  reads=[B_ptrO[p]], writes=[B_xT[i]])
                    kk.barrier()
                mx.close()
                with ExitStack() as ms:
                    build_tail(nc, kk, ms, sb, psum, sq, locals(), stage, dump)
                    kk.barrier()

        kk.barrier()
        E = kk.eng["sp"]
        for key, val_ in kk.out_dmas:
            if E.known.get(key, 0) < val_:
                E.h.wait_ge(kk._semof(key), val_)
                E.known[key] = val_
    return nc, dbg_out


def build_nsa(nc, kk, ns, sb, psum, sq, L, stage, dump):
    xT, omix, gates = L["xT"], L["omix"], L["gates"]
    B_xT, B_om, B_gates, B_const = L["B_xT"], L["B_om"], L["B_gates"], L["B_const"]
    win_d, hcd = L["win_d"], L["hcd"]
    ident_f, mask4, ovl = L["ident_f"], L["mask4"], L["ovl"]
    TN, TC, B_TN, B_TC = L["TN"], L["TC"], L["B_TN"], L["B_TC"]
    w2k, w2v, B_cw = L["w2k"], L["w2v"], L["B_cw"]
    ck1_d, cv1_d, posk_d, posv_d = L["ck1_d"], L["cv1_d"], L["posk_d"], L["posv_d"]

    qa = [sb(f"qa{g}", [96, NT, 4, 128], BF16, ns) for g in range(2)]
    ksa = [sb(f"ksa{g}", [96, S], BF16, ns) for g in range(2)]
    kwT = [sb(f"kwT{g}", [64, S], BF16, ns) for g in range(2)]
    vsa = [sb(f"vsa{g}", [128, NT, 65], BF16, ns) for g in range(2)]
    vwa = [sb(f"vwa{g}", [128, NT, 65], BF16, ns) for g in range(2)]
    kcmp = [sb(f"kcmp{g}", [64, 127], BF16, ns) for g in range(2)]
    vcmp = [sb(f"vcmp{g}", [127, 64], BF16, ns) for g in range(2)]
    imp = [sb(f"imp{g}", [128, NT, 32], F32, ns) for g in range(2)]
    B_kcmp = [kk.buf() for _ in range(2)]
    B_vcmp = [kk.buf() for _ in range(2)]
    B_imp = [kk.buf() for _ in range(2)]
    B_qa = [[kk.buf(f"qa{g}_{i}") for i in range(NT)] for g in range(2)]
    B_qm = [[kk.buf(f"qm{g}_{i}") for i in range(NT)] for g in range(2)]
    B_ks = [kk.buf() for g in range(2)]
    B_kw = [kk.buf() for g in range(2)]
    B_kc = [kk.buf() for g in range(2)]
    B_vc = [kk.buf() for g in range(2)]
    B_vs = [kk.buf() for g in range(2)]
    B_vw = [kk.buf() for g in range(2)]
    B_ee = kk.buf()
    for g in range(2):
        kk.dma("pool", ksa[g][64:96, :], hcd["eexp"].ap(), writes=[B_ee])
        kk.op("pool", lambda e, g=g: e.memset(vsa[g][:, :, 64:65], 1.0), writes=[B_vs[g]])
        kk.op("pool", lambda e, g=g: e.memset(vwa[g][:, :, 64:65], 1.0), writes=[B_vw[g]])
    mid = ns.enter_context(ExitStack())
    kvc = [sb(f"kvc{g}", [128, S], BF16, mid) for g in range(2)]
    ts0 = mid.enter_context(ExitStack())
    wA, B_wA = L["W1"], L["B_W1"]

    if True:
        ts = ts0
        pp = [psum(f"ppA{i}", [128, 512], F32, ts) for i in range(4)]
        B_pp = [kk.pbuf() for _ in range(4)]
        cnt = [0]

        def fm_pair(col0, dst_fns, dst_bufs_fns):
            for tg in range(4):
                p = cnt[0] % 4
                cnt[0] += 1
                for c in range(KC):
                    kk.op("pe", lambda e, p=p, c=c, tg=tg: e.matmul(
                        pp[p][:, :], lhsT=wA[:, c, col0:col0 + 128], rhs=xT[:, c, tg * 512:(tg + 1) * 512],
                        start=(c == 0), stop=(c == KC - 1)),
                        reads=[B_wA] + B_xT[tg * 4:tg * 4 + 4], writes=[B_pp[p]])
                for half in range(2):
                    dst = dst_fns[half](tg)
                    src = pp[p][64 * half:64 * half + 64, :].rearrange("p (a b) -> p a b", a=4)
                    same = (dst.base_partition() == 64 * half)
                    if same and half == 0:
                        kk.op("act", lambda e, dst=dst, src=src: e.copy(out=dst, in_=src),
                              reads=[B_pp[p]], writes=dst_bufs_fns[half](tg))
                    else:
                        kk.op("dve", lambda e, dst=dst, src=src: e.tensor_copy(out=dst, in_=src),
                              reads=[B_pp[p]], writes=dst_bufs_fns[half](tg))

        for g in range(2):
            for rp in range(2):
                hh = 4 * g + 2 * rp
                fm_pair(C_QA + hh * 64,
                        [lambda tg, g=g, r=2 * rp: qa[g][0:64, tg * 4:tg * 4 + 4, r, :],
                         lambda tg, g=g, r=2 * rp + 1: qa[g][0:64, tg * 4:tg * 4 + 4, r, :]],
                        [lambda tg, g=g: B_qa[g][tg * 4:tg * 4 + 4], lambda tg, g=g: B_qa[g][tg * 4:tg * 4 + 4]])
        for (c0, dst, bb, r0) in ((C_KC, kvc, B_kc, 0), (C_VC, kvc, B_vc, 64), (C_KS, ksa, B_ks, 0), (C_KW, kwT, B_kw, 0)):
            fm_pair(c0,
                    [lambda tg, dst=dst, r0=r0, g=g_: dst[g][r0:r0 + 64, tg * 512:(tg + 1) * 512].rearrange("p (a b) -> p a b", a=4)
                     for g_ in range(2)],
                    [lambda tg, bb=bb, g=g_: [bb[g]] for g_ in range(2)])
        for i in range(NT):
            p = cnt[0] % 4
            cnt[0] += 1
            for c in range(KC):
                kk.op("pe", lambda e, p=p, c=c, i=i: e.matmul(
                    pp[p][:, 0:128], lhsT=xT[:, c, i * 128:(i + 1) * 128], rhs=wA[:, c, C_VS:C_VS + 128],
                    start=(c == 0), stop=(c == KC - 1)), reads=[B_wA, B_xT[i]], writes=[B_pp[p]])
            for c in range(KC):
                kk.op("pe", lambda e, p=p, c=c, i=i: e.matmul(
                    pp[p][:, 128:280], lhsT=xT[:, c, i * 128:(i + 1) * 128], rhs=wA[:, c, C_VW:C_VW + 152],
                    start=(c == 0), stop=(c == KC - 1)), reads=[B_wA, B_xT[i]], writes=[B_pp[p]])
            for g in range(2):
                kk.op("dve", lambda e, p=p, g=g, i=i: e.tensor_copy(out=vsa[g][:, i, 0:64], in_=pp[p][:, g * 64:(g + 1) * 64]),
                      reads=[B_pp[p]], writes=[B_vs[g]])
                kk.op("dve", lambda e, p=p, g=g, i=i: e.tensor_copy(out=vwa[g][:, i, 0:64], in_=pp[p][:, 128 + g * 64:128 + (g + 1) * 64]),
                      reads=[B_pp[p]], writes=[B_vw[g]])
            kk.op("act", lambda e, p=p, i=i: e.activation(out=gates[:, i, :], in_=pp[p][:, 256:280], func=AF.Sigmoid),
                  reads=[B_pp[p]], writes=[B_gates])
        L["load_W1"]("B")
        kk.barrier()
        ts0.close()
    if stage == 2:
        dump("qa0", qa[0][0:64], [64, NT, 4, 128], B_qa[0], BF16)
        dump("ks0", ksa[0][:], [96, S], [B_ks[0], B_ee], BF16)
        dump("vs1", vsa[1][:], [128, NT, 65], [B_vs[1]], BF16)
        dump("gates", gates[:], [128, NT, 24], [B_gates])
        return

    with ExitStack() as ts:
        w1 = sb("w1", [128, 32, 128], BF16, ts)
        posT = sb("posT", [128, 32], BF16, ts)
        cpos = sb("cpos", [128, 2], F32, ts)
        B_w1 = kk.buf()
        B_cpos = kk.buf()
        kk.dma("pool", w1[0:64], ck1_d.ap().rearrange("(j d) h -> d j h", d=64), writes=[B_w1])
        kk.dma("pool", w1[64:128], cv1_d.ap().rearrange("(j d) h -> d j h", d=64), writes=[B_w1])
        with nc.allow_non_contiguous_dma(reason="tiny pos transpose"):
            kk.dma("pool", posT[0:64, :], posk_d.ap().rearrange("j d -> d j"), writes=[B_w1])
            kk.dma("pool", posT[64:128, :], posv_d.ap().rearrange("j d -> d j"), writes=[B_w1])
        pc = [psum(f"pcm{i}", [128, 512], F32, ts) for i in range(2)]
        B_pc = [kk.pbuf() for _ in range(2)]
        zz = [sb(f"zz{i}", [128, 4, 127], F32, ts) for i in range(2)]
        gl = [sb(f"gl{i}", [128, 127], BF16, ts) for i in range(2)]
        B_zz = [kk.buf() for _ in range(2)]
        B_gl = [kk.buf() for _ in range(2)]
        for kv in range(2):
            r0 = 64 * kv
            for j in range(32):
                kk.op("pe", lambda e, kv=kv, r0=r0, j=j: e.matmul(
                    pc[0][:, 500 + kv:501 + kv], lhsT=w1[r0:r0 + 64, j, :], rhs=posT[r0:r0 + 64, j:j + 1],
                    start=(j == 0), stop=(j == 31)), reads=[B_w1], writes=[B_pc[0]])
        kk.op("dve", lambda e: e.tensor_copy(out=cpos[:], in_=pc[0][:, 500:502]), reads=[B_pc[0]], writes=[B_cpos])
        it = 0
        for g in range(2):
            for kv, (bsrc, w2) in enumerate(((B_kc, w2k), (B_vc, w2v))):
                p = it % 2
                it += 1
                r0 = 64 * kv
                for j in range(32):
                    kk.op("pe", lambda e, p=p, j=j, g=g, r0=r0: e.matmul(
                        pc[p][:, 0:127], lhsT=w1[r0:r0 + 64, j, :], rhs=kvc[g][r0:r0 + 64, j:j + 16 * 126 + 1:16],
                        start=(j == 0), stop=(j == 31)), reads=[B_w1, bsrc[g]], writes=[B_pc[p]])
                z = zz[p]
                kk.op("dve", lambda e, p=p, kv=kv, z=z: e.tensor_scalar_add(out=z[:, 0, :], in0=pc[p][:, 0:127],
                                                                          scalar1=cpos[:, kv:kv + 1]),
                      reads=[B_pc[p], B_cpos], writes=[B_zz[p]])
                kk.op("dve", lambda e, z=z: e.tensor_tensor(out=z[:, 1, :], in0=z[:, 0, :], in1=z[:, 0, :], op=ALU.mult),
                      reads=[B_zz[p]], writes=[B_zz[p]])
                kk.op("dve", lambda e, z=z: e.tensor_scalar(out=z[:, 1, :], in0=z[:, 1, :], scalar1=0.044715, scalar2=1.0,
                                                            op0=ALU.mult, op1=ALU.add), reads=[B_zz[p]], writes=[B_zz[p]])
                kk.op("dve", lambda e, z=z: e.tensor_tensor(out=z[:, 2, :], in0=z[:, 1, :], in1=z[:, 0, :], op=ALU.mult),
                      reads=[B_zz[p]], writes=[B_zz[p]])
                kk.op("act", lambda e, z=z: e.activation(out=z[:, 3, :], in_=z[:, 2, :], func=AF.Sigmoid,
                                                         scale=2.0 * math.sqrt(2.0 / math.pi)),
                      reads=[B_zz[p]], writes=[B_zz[p]])
                kk.op("dve", lambda e, z=z, p=p: e.tensor_tensor(out=gl[p][:], in0=z[:, 3, :], in1=z[:, 0, :], op=ALU.mult),
                      reads=[B_zz[p]], writes=[B_gl[p]])
                if kv == 0:
                    kk.op("pe", lambda e, p=p, w2=w2: e.matmul(pc[p][0:64, 128:255], lhsT=w2[:], rhs=gl[p][:],
                                                              start=True, stop=True),
                          reads=[B_cw, B_gl[p]], writes=[B_pc[p]])
                    kk.op("dve", lambda e, p=p, g=g: e.tensor_copy(out=kcmp[g][:], in_=pc[p][0:64, 128:255]),
                          reads=[B_pc[p]], writes=[B_kcmp[g]])
                else:
                    kk.op("pe", lambda e, p=p, w2=w2: e.matmul(pc[p][0:127, 256:320], lhsT=gl[p][:], rhs=w2[:],
                                                              start=True, stop=True),
                          reads=[B_cw, B_gl[p]], writes=[B_pc[p]])
                    kk.op("dve", lambda e, p=p, g=g: e.tensor_copy(out=vcmp[g][:], in_=pc[p][0:127, 256:320]),
                          reads=[B_pc[p]], writes=[B_vcmp[g]])
        kk.barrier()
    mid.close()
    if stage == 3:
        dump("kcmp0", kcmp[0][:], [64, 127], [B_kcmp[0]], BF16)
        dump("vcmp1", vcmp[1][:], [127, 64], [B_vcmp[1]], BF16)
        return

    with ExitStack() as ts:
        psc = [psum(f"psc{i}", [128, 4, 128], F32, ts) for i in range(2)]
        ptp = [psum(f"ptp{i}", [128, 4, 128], F32, ts) for i in range(2)]
        ptb = [psum(f"ptb{i}", [128, 8, 128], BF16, ts) for i in range(2)]
        pov = [psum(f"pov{i}", [128, 512], F32, ts) for i in range(2)]
        B_ptb = [kk.pbuf() for _ in range(2)]
        B_psc = [kk.pbuf() for _ in range(2)]
        B_ptp = [kk.pbuf() for _ in range(2)]
        B_pov = [kk.pbuf() for _ in range(2)]
        s_sb = [sb(f"s_sb{i}", [128, 4, 127], F32, ts) for i in range(2)]
        e_sb = [sb(f"e_sb{i}", [128, 4, 127], F32, ts) for i in range(2)]
        pT = [sb(f"pT{i}", [127, 4, 128], BF16, ts) for i in range(2)]
        pbf = [sb(f"pbf{i}", [128, 4, 127], BF16, ts) for i in range(2)]
        psm = [sb(f"psm{i}", [128, 128], F32, ts) for i in range(2)]
        B_pbf = [kk.buf() for _ in range(2)]
        B_psm = [kk.buf() for _ in range(2)]
        for i_ in range(2):
            kk.op("pool", lambda e, i_=i_: e.memset(psm[i_][:], 0.0), writes=[B_psm[i_]])
        stc = [sb(f"stc{i}", [128, 16], F32, ts) for i in range(2)]
        B_s = [kk.buf() for _ in range(2)]
        B_e = [kk.buf() for _ in range(2)]
        B_pT = [kk.buf() for _ in range(2)]
        B_stc = [kk.buf() for _ in range(2)]
        its = [(g, tb) for g in range(2) for tb in range(NT)]

        def cmp_p1a(k):
            g, tb = its[k]
            p = k % 2
            for r in range(4):
                kk.op("pe", lambda e, p=p, g=g, tb=tb, r=r: e.matmul(
                    psc[p][:, r, 0:127], lhsT=qa[g][0:64, tb, r, :], rhs=kcmp[g][:], start=True, stop=True),
                    reads=[B_qa[g][tb], B_kcmp[g]], writes=[B_psc[p]])
            off = 120 - 8 * tb
            kk.op("dve", lambda e, p=p, g=g, off=off: e.tensor_tensor(
                out=s_sb[p][:], in0=psc[p][:, :, 0:127], in1=TC[:, g, :, off:off + 127], op=ALU.add),
                reads=[B_psc[p], B_TC], writes=[B_s[p]])
            for r in range(4):
                kk.op("act", lambda e, p=p, r=r: e.activation(out=e_sb[p][:, r, :], in_=s_sb[p][:, r, :], func=AF.Exp,
                                                             scale=SC_A, accum_out=stc[p][:, r:r + 1]),
                      reads=[B_s[p]], writes=[B_e[p], B_stc[p]])

        def cmp_p1b(k):
            g, tb = its[k]
            p = k % 2
            kk.op("dve", lambda e, p=p: e.tensor_scalar_max(out=stc[p][:, 4:8], in0=stc[p][:, 0:4], scalar1=TINY),
                  reads=[B_stc[p]], writes=[B_stc[p]])
            kk.op("dve", lambda e, p=p: e.reciprocal(out=stc[p][:, 8:12], in_=stc[p][:, 4:8]),
                  reads=[B_stc[p]], writes=[B_stc[p]])
            kk.op("pool", lambda e, p=p: e.tensor_tensor(
                out=pbf[p][:], in0=e_sb[p][:], in1=stc[p][:, 8:12].unsqueeze(2).to_broadcast([128, 4, 127]), op=ALU.mult),
                reads=[B_e[p], B_stc[p]], writes=[B_pbf[p]])
            kk.op("dve", lambda e, p=p: e.tensor_scalar_mul(out=psm[p][:, 0:127], in0=e_sb[p][:, 0, :], scalar1=stc[p][:, 8:9]),
                  reads=[B_e[p], B_stc[p]], writes=[B_psm[p]])
            for r in range(1, 4):
                kk.op("dve", lambda e, p=p, r=r: e.scalar_tensor_tensor(
                    out=psm[p][:, 0:127], in0=e_sb[p][:, r, :], scalar=stc[p][:, 8 + r:9 + r], in1=psm[p][:, 0:127],
                    op0=ALU.mult, op1=ALU.add), reads=[B_e[p], B_stc[p], B_psm[p]], writes=[B_psm[p]])
            kk.op("dve", lambda e, p=p, g=g, tb=tb: e.reduce_sum(out=imp[g][:, tb, :], in_=psm[p][:].rearrange("p (j i) -> p j i", i=4),
                                                             axis=AX.X), reads=[B_psm[p]], writes=[B_imp[g]])
            kk.op("dve", lambda e, p=p, g=g, tb=tb: e.tensor_tensor(out=imp[g][:, tb, 1:32], in0=imp[g][:, tb, 1:32],
                                                                in1=psm[p][:, 3:127:4], op=ALU.add),
                  reads=[B_psm[p], B_imp[g]], writes=[B_imp[g]])

        def cmp_p2(k):
            p = k % 2
            for r in range(4):
                kk.op("pe", lambda e, p=p, r=r: e.transpose(ptb[p][0:127, r, :], pbf[p][:, r, :], L["ident_b"][:]),
                      reads=[B_pbf[p], B_const], writes=[B_ptb[p]])
            kk.op("act", lambda e, p=p: e.copy(out=pT[p][:], in_=ptb[p][0:127, 0:4, :]), reads=[B_ptb[p]], writes=[B_pT[p]])

        def cmp_p3(k):
            g, tb = its[k]
            p = k % 2
            for r in range(4):
                kk.op("pe", lambda e, p=p, r=r, g=g: e.matmul(pov[p][:, r * 64:(r + 1) * 64], lhsT=pT[p][:, r, :],
                                                              rhs=vcmp[g][:], start=True, stop=True),
                      reads=[B_pT[p], B_vcmp[g]], writes=[B_pov[p]])
            kk.op("dve", lambda e, p=p, g=g, tb=tb: e.tensor_tensor(
                out=omix[:, tb, g * 256:(g + 1) * 256].rearrange("p (r d) -> p r d", r=4),
                in0=pov[p][:, 0:256].rearrange("p (r d) -> p r d", r=4),
                in1=gates[:, tb, g * 12:(g + 1) * 12].rearrange("p (r b) -> p r b", b=3)[:, :, 0:1].to_broadcast([128, 4, 64]),
                op=ALU.mult), reads=[B_pov[p], B_gates], writes=[B_om[tb]])

        n_it = len(its)
        for k in range(n_it + 3):
            if k < n_it:
                cmp_p1a(k)
            if 0 <= k - 1 < n_it:
                cmp_p1b(k - 1)
            if 0 <= k - 2 < n_it:
                cmp_p2(k - 2)
            if 0 <= k - 3 < n_it:
                cmp_p3(k - 3)
        nfv = sb("nfv", [128, 16, 32], F32, ts)
        addc = sb("addc", [128, 16, 32], F32, ts)
        validt = sb("validt", [128, 16, 32], F32, ts)
        kk.dma("sp", nfv[:], hcd["nfv"].ap(), writes=[B_const])
        kk.dma("sp", addc[:], hcd["addc"].ap(), writes=[B_const])
        kk.dma("sp", validt[:], hcd["valid"].ap(), writes=[B_const])
        sc_t = sb("sc_t", [128, NT, 32], F32, ts)
        m8 = sb("m8", [128, NT, 8], F32, ts)
        stg = [sb(f"stg{i}", [128, 96], F32, ts) for i in range(2)]
        B_sc = kk.buf()
        B_m8 = kk.buf()
        B_stg = [kk.buf() for _ in range(2)]
        for i_ in range(2):
            kk.op("pool", lambda e, i_=i_: e.memset(stg[i_][:], 0.0), writes=[B_stg[i_]])
        for g in range(2):
            kk.op("dve", lambda e, g=g: e.tensor_tensor(out=sc_t[:], in0=imp[g][:], in1=nfv[:], op=ALU.mult),
                  reads=[B_imp[g], B_const], writes=[B_sc])
            kk.op("dve", lambda e: e.tensor_tensor(out=sc_t[:], in0=sc_t[:], in1=addc[:], op=ALU.add),
                  reads=[B_sc, B_const], writes=[B_sc])
            for tb in range(NT):
                kk.op("dve", lambda e, tb=tb: e.max(out=m8[:, tb, :], in_=sc_t[:, tb, :]), reads=[B_sc], writes=[B_m8])
            kk.op("dve", lambda e: e.tensor_tensor(out=sc_t[:], in0=sc_t[:], in1=m8[:, :, 7:8].to_broadcast([128, NT, 32]),
                                                   op=ALU.is_ge), reads=[B_sc, B_m8], writes=[B_sc])
            kk.op("dve", lambda e: e.tensor_tensor(out=sc_t[:], in0=sc_t[:], in1=validt[:], op=ALU.mult),
                  reads=[B_sc, B_const], writes=[B_sc])
            kk.op("dve", lambda e: e.tensor_scalar(out=sc_t[:], in0=sc_t[:], scalar1=-1.0, scalar2=-NEGB,
                                                   op0=ALU.add, op1=ALU.mult), reads=[B_sc], writes=[B_sc])
            for tb in range(NT):
                p = tb % 2
                kk.op("dve", lambda e, p=p, tb=tb: e.tensor_copy(out=stg[p][:, 64:96], in_=sc_t[:, tb, :]),
                      reads=[B_sc], writes=[B_stg[p]])
                kk.op("pe", lambda e, p=p, tb=tb: e.transpose(ptp[p][0:96, 0, :], stg[p][:], ident_f[:]),
                      reads=[B_stg[p], B_const], writes=[B_ptp[p]])
                kk.op("act", lambda e, p=p, g=g, tb=tb: e.copy(
                    out=qa[g][64:96, tb, :, :], in_=ptp[p][64:96, 0:1, :].to_broadcast([32, 4, 128])),
                    reads=[B_ptp[p]], writes=[B_qm[g][tb]])
        kk.barrier()
    if stage == 4 and False:
        return

    with ExitStack() as ts:
        NSB = 3
        pss = [psum(f"pss{i}", [128, 2, 512], F32, ts) for i in range(NSB)]
        po_t = [psum(f"po{i}", [128, 512], F32, ts) for i in range(2)]
        po = [t_[:, 0:260].rearrange("p (c d) -> p c d", d=65) for t_ in po_t]
        B_pss = [kk.pbuf() for _ in range(NSB)]
        B_po = [kk.pbuf() for _ in range(4)]
        NPT = 4
        pt = [sb(f"ptS{i}", [128, 2, 512], BF16, ts) for i in range(NPT)]
        B_pt = [kk.buf() for _ in range(NPT)]
        fac = [sb(f"fac{i}", [128, 16], F32, ts) for i in range(2)]
        B_fac = [kk.buf() for _ in range(2)]
        ocs = [sb(f"ocs{i}", [128, 2, 4, 65], F32, ts) for i in range(2)]
        B_ocs = [[kk.buf() for _ in range(2)] for _ in range(2)]
        oacc = [sb(f"oacc{i}", [128, 4, 64], F32, ts) for i in range(2)]
        B_oacc = [kk.buf() for _ in range(2)]
        tiles = []
        it = 0
        for g in range(2):
            for tb in range(NT):
                par = it % 2
                it += 1
                for br in range(2):
                    kbs = list(range(0, tb + 1)) if br == 0 else list(range(max(0, tb - 4), tb + 1))
                    for u0 in range(0, len(kbs), 2):
                        tiles.append((g, tb, par, br, kbs[u0:u0 + 2], kbs))

        def sw_S(idx):
            g, tb, par, br, ukbs, kbs = tiles[idx]
            p = idx % NSB
            q = idx % NPT
            ns = len(ukbs)
            for sl, kb in enumerate(ukbs):
                if br == 0:
                    kk.op("pe", lambda e, sl=sl, kb=kb: e.matmul(
                        pss[p][:, sl, :], lhsT=ksa[g][0:96, kb * 128:(kb + 1) * 128],
                        rhs=qa[g][0:96, tb, :, :].rearrange("p r t -> p (r t)"), start=True, stop=True),
                        reads=[B_ks[g], B_ee, B_qa[g][tb], B_qm[g][tb]], writes=[B_pss[p]])
                else:
                    kk.op("pe", lambda e, sl=sl, kb=kb: e.matmul(
                        pss[p][:, sl, :], lhsT=kwT[g][0:64, kb * 128:(kb + 1) * 128],
                        rhs=qa[g][0:64, tb, :, :].rearrange("p r t -> p (r t)"), start=True, stop=True),
                        reads=[B_kw[g], B_qa[g][tb]], writes=[B_pss[p]])
            for sl, kb in enumerate(ukbs):
                if kb == tb or kb == tb - 1:
                    seg = 0 if kb == tb else 1
                    kk.op("dve", lambda e, sl=sl, seg=seg: e.tensor_tensor(
                        out=pss[p][:, sl, :], in0=pss[p][:, sl, :], in1=TN[:, g, seg, :, :].rearrange("p r t -> p (r t)"),
                        op=ALU.add), reads=[B_pss[p], B_TN], writes=[B_pss[p]])
                elif br == 1 and kb == tb - 4:
                    kk.op("dve", lambda e, sl=sl: e.tensor_tensor(
                        out=pss[p][:, sl, :].rearrange("p (r t) -> p r t", r=4),
                        in0=pss[p][:, sl, :].rearrange("p (r t) -> p r t", r=4),
                        in1=mask4[:].unsqueeze(1).to_broadcast([128, 4, 128]), op=ALU.add),
                        reads=[B_pss[p], B_const], writes=[B_pss[p]])
            kk.op("act", lambda e: e.activation(out=pt[q][:, 0:ns, :], in_=pss[p][:, 0:ns, :], func=AF.Exp, scale=SC_A),
                  reads=[B_pss[p]], writes=[B_pt[q]])

        def sw_PV(idx):
            g, tb, par, br, ukbs, kbs = tiles[idx]
            q = idx % NPT
            pob = po[br]
            B_pob = B_po[br]
            va = vsa if br == 0 else vwa
            Bv = B_vs if br == 0 else B_vw
            if ukbs[0] == kbs[0]:
                kk.drain(until_tag=("po", br))
            for sl, kb in enumerate(ukbs):
                for r in range(4):
                    kk.op("pe", lambda e, r=r, sl=sl, kb=kb: e.matmul(
                        pob[:, r, :], lhsT=pt[q][:, sl, r * 128:(r + 1) * 128], rhs=va[g][:, kb, :],
                        start=(kb == kbs[0] and r == 0), stop=(kb == kbs[-1]), skip_group_check=True),
                        reads=[B_pt[q], Bv[g]], writes=[B_pob])
            if ukbs[-1] != kbs[-1]:
                return
            f = fac[par]
            bidx = 1 if br == 0 else 2
            tag = ("po", br)
            oc_ = ocs[par][:, br]
            B_oc_ = B_ocs[par][br]
            kk.defer("dve", lambda e: e.tensor_copy(out=oc_, in_=pob), reads=[B_pob], writes=[B_oc_], tag=tag)
            kk.defer("dve", lambda e: e.tensor_scalar_max(
                out=f[:, br * 8:br * 8 + 4].unsqueeze(2), in0=oc_[:, :, 64:65], scalar1=TINY),
                reads=[B_oc_], writes=[B_fac[par]])
            kk.defer("dve", lambda e: e.reciprocal(out=f[:, br * 8 + 4:br * 8 + 8], in_=f[:, br * 8:br * 8 + 4]),
                     reads=[B_fac[par]], writes=[B_fac[par]])
            kk.defer("dve", lambda e: e.tensor_tensor(
                out=f[:, br * 8 + 4:br * 8 + 8].unsqueeze(2), in0=f[:, br * 8 + 4:br * 8 + 8].unsqueeze(2),
                in1=gates[:, tb, g * 12:(g + 1) * 12].rearrange("p (r b) -> p r b", b=3)[:, :, bidx:bidx + 1],
                op=ALU.mult), reads=[B_fac[par], B_gates], writes=[B_fac[par]])
            if br == 0:
                kk.defer("dve", lambda e: e.tensor_tensor(
                    out=oacc[par][:], in0=oc_[:, :, 0:64], in1=f[:, 4:8].unsqueeze(2).to_broadcast([128, 4, 64]),
                    op=ALU.mult), reads=[B_oc_, B_fac[par]], writes=[B_oacc[par]])
                kk.defer("dve", lambda e: e.tensor_tensor(
                    out=oacc[par][:], in0=oacc[par][:],
                    in1=omix[:, tb, g * 256:(g + 1) * 256].rearrange("p (r d) -> p r d", r=4), op=ALU.add),
                    reads=[B_oacc[par], B_om[tb]], writes=[B_oacc[par]])
            else:
                kk.defer("dve", lambda e: e.tensor_tensor(
                    out=oc_[:, :, 0:64], in0=oc_[:, :, 0:64], in1=f[:, 12:16].unsqueeze(2).to_broadcast([128, 4, 64]),
                    op=ALU.mult), reads=[B_oc_, B_fac[par]], writes=[B_oc_])
                kk.defer("dve", lambda e: e.tensor_tensor(
                    out=omix[:, tb, g * 256:(g + 1) * 256].rearrange("p (r d) -> p r d", r=4),
                    in0=oc_[:, :, 0:64], in1=oacc[par][:], op=ALU.add),
                    reads=[B_oc_, B_oacc[par]], writes=[B_om[tb]])

        LA = 2
        for idx in range(len(tiles) + LA):
            if idx < len(tiles):
                sw_S(idx)
            if idx - LA >= 0:
                sw_PV(idx - LA)
            kk.drain(2)
        kk.drain()
        kk.barrier()


def build_diff(nc, kk, ds, sb, psum, sq, L, stage, dump):
    xT, omix = L["xT"], L["omix"]
    B_xT, B_om, B_const = L["B_xT"], L["B_om"], L["B_const"]
    win_d = L["win_d"]
    TD, B_TD, neglam, B_l, subln = L["TD"], L["B_TD"], L["neglam"], L["B_l"], L["subln"]

    qb = sb("qbT", [128, 4, S], BF16, ds)
    kb_ = sb("kbT", [128, 4, S], BF16, ds)
    vb = sb("vb", [128, NT, 8, 65], BF16, ds)
    B_qb = [[kk.buf() for _ in range(4)] for _ in range(4)]
    B_kb = [kk.buf() for _ in range(4)]
    B_vb = kk.buf()
    kk.op("pool", lambda e: e.memset(vb[:, :, :, 64:65], 1.0), writes=[B_vb])
    NCOL = 1536
    wv = win_d.ap().rearrange("(kc p) c -> p kc c", p=128)

    with ExitStack() as ts:
        wB, B_wB = L["W1"], L["B_W1"]
        pp = [psum(f"ppB{i}", [128, 512], F32, ts) for i in range(4)]
        B_pp = [kk.pbuf() for _ in range(4)]
        cnt = 0
        for which, dst in ((0, qb), (1, kb_)):
            for ch in range(4):
                col0 = which * 512 + ch * 128
                for tg in range(4):
                    p = cnt % 4
                    cnt += 1
                    for c in range(KC):
                        kk.op("pe", lambda e, p=p, c=c, tg=tg, col0=col0: e.matmul(
                            pp[p][:, :], lhsT=wB[:, c, col0:col0 + 128], rhs=xT[:, c, tg * 512:(tg + 1) * 512],
                            start=(c == 0), stop=(c == KC - 1)),
                            reads=[B_wB] + B_xT[tg * 4:tg * 4 + 4], writes=[B_pp[p]])
                    bw = [B_qb[ch][tg]] if which == 0 else [B_kb[ch]]
                    if cnt % 2 == 0:
                        kk.op("act", lambda e, p=p, dst=dst, ch=ch, tg=tg: e.copy(out=dst[:, ch, tg * 512:(tg + 1) * 512], in_=pp[p][:, :]),
                              reads=[B_pp[p]], writes=bw)
                    else:
                        kk.op("dve", lambda e, p=p, dst=dst, ch=ch, tg=tg: e.tensor_copy(out=dst[:, ch, tg * 512:(tg + 1) * 512], in_=pp[p][:, :]),
                              reads=[B_pp[p]], writes=bw)
        for i in range(NT):
            p = cnt % 4
            cnt += 1
            for c in range(KC):
                kk.op("pe", lambda e, p=p, c=c, i=i: e.matmul(
                    pp[p][:, :], lhsT=xT[:, c, i * 128:(i + 1) * 128], rhs=wB[:, c, 1024:1536],
                    start=(c == 0), stop=(c == KC - 1)), reads=[B_wB, B_xT[i]], writes=[B_pp[p]])
            kk.op("dve", lambda e, p=p, i=i: e.tensor_copy(out=vb[:, i, :, 0:64], in_=pp[p][:, :].rearrange("p (h d) -> p h d", h=8)),
                  reads=[B_pp[p]], writes=[B_vb])
        L["load_W1"]("O")
        kk.barrier()

    with ExitStack() as ts:
        NSB = 4
        NS2 = 3
        pss2 = [psum(f"psd{i}", [128, 2, 512], F32, ts) for i in range(NS2)]
        po_t = [psum(f"pod{i}", [128, 512], F32, ts) for i in range(2)]
        po = [t_[:, 0:260].rearrange("p (c d) -> p c d", d=65) for t_ in po_t]
        B_pss = [kk.pbuf() for _ in range(NSB)]
        B_po = [kk.pbuf() for _ in range(4)]
        NPT = 6
        NPT = 4
        pt = [sb(f"ptD{i}", [128, 2, 512], BF16, ts) for i in range(NPT)]
        B_pt = [kk.buf() for _ in range(NPT)]
        fac = [sb(f"facD{i}", [128, 24], F32, ts) for i in range(2)]
        B_fac = [kk.buf() for _ in range(2)]
        oc = [sb(f"ocD{i}", [128, 2, 4, 65], F32, ts) for i in range(2)]
        B_oc = [kk.buf() for _ in range(2)]
        o1 = [sb(f"o1D{i}", [128, 4, 64], F32, ts) for i in range(2)]
        o2 = [sb(f"o2D{i}", [128, 4, 64], F32, ts) for i in range(2)]
        B_o1 = [kk.buf() for _ in range(2)]
        B_o2 = [kk.buf() for _ in range(2)]
        tiles = []
        it = 0
        for h in range(8):
            for G in range(4):
                par = it % 2
                it += 1
                for kb in range(4 * G + 4):
                    tiles.append((h, G, par, kb))

        def d_S(idx):
            h, G, par, kb = tiles[idx]
            ch = h // 2
            j = max(0, kb - 4 * G)
            n0 = 128 * j
            N = 512 - n0
            p = idx % NS2
            q = idx % NPT
            for mp in range(2):
                base = (h % 2) * 64 + mp * 32
                kw = {"tile_position": (base, 0)} if base == 96 else {}
                kk.op("pe", lambda e, mp=mp, base=base, kw=kw: e.matmul(
                    pss2[p][:, mp, 0:N], lhsT=kb_[base:base + 32, ch, kb * 128:(kb + 1) * 128],
                    rhs=qb[base:base + 32, ch, G * 512 + n0:(G + 1) * 512], start=True, stop=True, **kw),
                    reads=[B_kb[ch], B_qb[ch][G]], writes=[B_pss[p]])
            if kb >= 4 * G:
                w = min(256, N)
                kk.op("dve", lambda e: e.tensor_tensor(
                    out=pss2[p][:, :, 0:w], in0=pss2[p][:, :, 0:w], in1=TD[:, h, 0:w].unsqueeze(1).to_broadcast([128, 2, w]),
                    op=ALU.add), reads=[B_pss[p], B_TD], writes=[B_pss[p]])
            elif kb == 4 * G - 1:
                kk.op("dve", lambda e: e.tensor_tensor(
                    out=pss2[p][:, :, 0:128], in0=pss2[p][:, :, 0:128],
                    in1=TD[:, h, 128:256].unsqueeze(1).to_broadcast([128, 2, 128]), op=ALU.add),
                    reads=[B_pss[p], B_TD], writes=[B_pss[p]])
            kk.op("act", lambda e: e.activation(out=pt[q][:, :, 0:N], in_=pss2[p][:, :, 0:N], func=AF.Exp, scale=SC_B),
                  reads=[B_pss[p]], writes=[B_pt[q]])

        def d_PV(idx):
            h, G, par, kb = tiles[idx]
            nkb = 4 * G + 4
            j = max(0, kb - 4 * G)
            q = idx % NPT
            for mp in range(2):
                pob = po[mp]
                B_pob = B_po[mp]
                if kb == 0:
                    kk.drain(until_tag=("pod", mp))
                for c in range(j, 4):
                    kbl = 4 * G + c
                    kk.op("pe", lambda e, c=c, kbl=kbl, mp=mp, pob=pob: e.matmul(
                        pob[:, c, :], lhsT=pt[q][:, mp, (c - j) * 128:(c - j + 1) * 128], rhs=vb[:, kb, h, :],
                        start=(kb == 0 and c == 0), stop=(kb == kbl), skip_group_check=True),
                        reads=[B_pt[q], B_vb], writes=[B_pob])
            if kb != nkb - 1:
                return

            f = fac[par]
            p0, p1 = oc[par][:, 0], oc[par][:, 1]
            Bp0 = Bp1 = B_oc[par]
            kk.defer("dve", lambda e: e.tensor_copy(out=oc[par][:, 0], in_=po[0]),
                     reads=[B_po[0]], writes=[B_oc[par]], tag=("pod", 0))
            kk.defer("dve", lambda e: e.tensor_copy(out=oc[par][:, 1], in_=po[1]),
                     reads=[B_po[1]], writes=[B_oc[par]], tag=("pod", 1))
            kk.defer("dve", lambda e: e.tensor_scalar_max(out=f[:, 0:8].rearrange("p (m c) -> p m c", m=2).unsqueeze(3),
                                                          in0=oc[par][:, :, :, 64:65], scalar1=TINY),
                     reads=[B_oc[par]], writes=[B_fac[par]])
            kk.defer("dve", lambda e: e.reciprocal(out=f[:, 8:16], in_=f[:, 0:8]), reads=[B_fac[par]], writes=[B_fac[par]])
            kk.defer("dve", lambda e: e.tensor_scalar_mul(out=f[:, 12:16], in0=f[:, 12:16], scalar1=neglam[:, 0:1]),
                     reads=[B_fac[par], B_l], writes=[B_fac[par]])
            kk.defer("dve", lambda e: e.tensor_tensor(
                out=o1[par][:], in0=p0[:, :, 0:64], in1=f[:, 8:12].unsqueeze(2).to_broadcast([128, 4, 64]), op=ALU.mult),
                reads=[Bp0, B_fac[par]], writes=[B_o1[par]])
            kk.defer("dve", lambda e: e.tensor_tensor(
                out=o2[par][:], in0=p1[:, :, 0:64], in1=f[:, 12:16].unsqueeze(2).to_broadcast([128, 4, 64]), op=ALU.mult),
                reads=[Bp1, B_fac[par]], writes=[B_o2[par]])
            kk.defer("dve", lambda e: e.tensor_tensor(out=o1[par][:], in0=o1[par][:], in1=o2[par][:], op=ALU.add),
                     reads=[B_o1[par], B_o2[par]], writes=[B_o1[par]])
            kk.defer("dve", lambda e: e.tensor_tensor(out=o2[par][:], in0=o1[par][:], in1=o1[par][:], op=ALU.mult),
                     reads=[B_o1[par]], writes=[B_o2[par]])
            kk.defer("dve", lambda e: e.reduce_sum(out=f[:, 16:20], in_=o2[par][:], axis=AX.X),
                     reads=[B_o2[par]], writes=[B_fac[par]])
            kk.defer("dve", lambda e: e.tensor_scalar(out=f[:, 20:24], in0=f[:, 16:20], scalar1=1.0 / 64, scalar2=EPS,
                                                      op0=ALU.mult, op1=ALU.add), reads=[B_fac[par]], writes=[B_fac[par]])
            kk.defer("act", lambda e: e.activation(out=f[:, 20:24], in_=f[:, 20:24], func=AF.Ln),
                     reads=[B_fac[par]], writes=[B_fac[par]])
            kk.defer("act", lambda e: e.activation(out=f[:, 16:20], in_=f[:, 20:24], func=AF.Exp, scale=-0.5),
                     reads=[B_fac[par]], writes=[B_fac[par]])
            kk.defer("dve", lambda e: e.tensor_tensor(
                out=o1[par][:], in0=o1[par][:], in1=f[:, 16:20].unsqueeze(2).to_broadcast([128, 4, 64]), op=ALU.mult),
                reads=[B_o1[par], B_fac[par]], writes=[B_o1[par]])
            kk.defer("dve", lambda e: e.scalar_tensor_tensor(
                out=omix[:, 4 * G:4 * G + 4, 512 + h * 64:512 + (h + 1) * 64], in0=o1[par][:], scalar=1.0 - LAMBDA_INIT,
                in1=subln[:].unsqueeze(1).to_broadcast([128, 4, 64]), op0=ALU.mult, op1=ALU.mult),
                reads=[B_o1[par], B_const], writes=B_om[4 * G:4 * G + 4])

        LA = 2
        for idx in range(len(tiles) + LA):
            if idx < len(tiles):
                d_S(idx)
            if idx - LA >= 0:
                d_PV(idx - LA)
            kk.drain(2)
        kk.drain()
        kk.barrier()


def build_tail(nc, kk, ms, sb, psum, sq, L, stage, dump):
    xT, omix = L["xT"], L["omix"]
    B_xT, B_om, B_const = L["B_xT"], L["B_om"], L["B_const"]
    x_d, out_d, wout_d, wg_d, wu_d, wd_d = L["x_d"], L["out_d"], L["wout_d"], L["wg_d"], L["wu_d"], L["wd_d"]
    ident_f, ident_b, wr, rbias = L["ident_f"], L["ident_b"], L["wr"], L["rbias"]
    lnffn_d, lnfin_d, bcast_rows = L["lnffn_d"], L["lnfin_d"], L["bcast_rows"]

    hres = sb("hres", [128, NT, D], F32, ms)
    B_h = [kk.buf(f"h{i}") for i in range(NT)]
    comb = sb("comb", [128, NT, 32], F32, ms)
    B_comb = kk.buf("comb")

    NWB = 2
    wgu = [sb("wgu0", [128, 2, KC, 512], BF16, ms)]
    wdn = [sb("wdn0", [128, 2, 2, D], BF16, ms)]
    B_wgu = [kk.buf() for _ in range(NWB)]
    B_wdn = [kk.buf() for _ in range(NWB)]

    def load_pair(pr, nobarrier=False):
        w = pr % NWB
        for ei in range(2):
            e_ = 2 * pr + ei
            kk.dma("pool", wgu[w][:, ei, :, 0:256], wg_d.ap()[e_].rearrange("(kc p) f -> p kc f", p=128), writes=[B_wgu[w]], nobarrier=nobarrier)
            kk.dma("pool", wgu[w][:, ei, :, 256:512], wu_d.ap()[e_].rearrange("(kc p) f -> p kc f", p=128), writes=[B_wgu[w]], nobarrier=nobarrier)
            kk.dma("pool", wdn[w][:, ei, :, :], wd_d.ap()[e_].rearrange("(fc p) d -> p fc d", p=128), writes=[B_wdn[w]], nobarrier=nobarrier)

    if stage > 7:
        load_pair(0, nobarrier=True)

    with ExitStack() as ts:
        wo, B_wo = L["W1"], L["B_W1"]
        py = [psum(f"pyO{i}", [128, D], F32, ts) for i in range(2)]
        B_py = [kk.pbuf() for _ in range(2)]
        for i in range(NT):
            kk.dma("sp", hres[:, i, :], x_d.ap()[sq, i * 128:(i + 1) * 128, :], writes=[B_h[i]])
        for i in range(NT):
            p = i % 2
            for hf in range(2):
                for c in range(KC):
                    kk.op("pe", lambda e, p=p, c=c, i=i, hf=hf: e.matmul(
                        py[p][:, hf * 512:(hf + 1) * 512], lhsT=xT[:, c, i * 128:(i + 1) * 128],
                        rhs=wo[:, c, hf * 512:(hf + 1) * 512], start=(c == 0), stop=(c == KC - 1)),
                        reads=[B_xT[i], B_wo], writes=[B_py[p]])
            kk.op("dve", lambda e, p=p, i=i: e.tensor_tensor(out=hres[:, i, :], in0=hres[:, i, :], in1=py[p][:], op=ALU.add),
                  reads=[B_py[p], B_h[i]], writes=[B_h[i]])
        if sq + 1 < L["nseq"]:
            L["load_W1"]("A")
        kk.barrier()
    if stage == 6:
        dump("h1", hres[:], [128, NT, D], B_h)
        return

    lg = sb("lg", [128, NT, 36], F32, ms)
    B_lg = kk.buf()
    with ExitStack() as ts:
        gffn = sb("gffn", [128, D], F32, ts)
        B_gf = kk.buf()
        kk.dma("sp", gffn[:], bcast_rows(lnffn_d, D), writes=[B_gf])
        NB1 = 2
        tn = [sb(f"tn{i}", [128, D], F32, ts) for i in range(NB1)]
        tTf = [sb(f"tTf{i}", [128, KC, 128], F32, ts) for i in range(2)]
        st = sb("stM", [128, NT, 4], F32, ts)
        ptr = [psum(f"ptrM{i}", [128, KC, 128], F32, ts) for i in range(2)]
        plg = [psum(f"plg{i}", [128, 512], F32, ts) for i in range(2)]
        B_tn = [kk.buf() for _ in range(NB1)]
        B_tTf = [kk.buf() for _ in range(2)]
        B_ptr = [kk.pbuf() for _ in range(2)]
        B_plg = [kk.pbuf() for _ in range(2)]
        B_st = [kk.buf() for _ in range(NT)]
        B_junk = kk.buf()

        B_stall = kk.buf()
        for i in range(NT):
            jb, Bj = tn[i % NB1], B_tn[i % NB1]
            kk.op("act", lambda e, i=i, jb=jb: e.activation(out=jb[:], in_=hres[:, i, :], func=AF.Square,
                                                            accum_out=st[:, i, 0:1]),
                  reads=[B_h[i]], writes=[B_stall, Bj])
        kk.op("act", lambda e: e.activation(out=st[:, :, 1:2], in_=st[:, :, 0:1], func=AF.Sqrt, bias=EPS, scale=1.0 / D),
              reads=[B_stall], writes=[B_stall])
        kk.op("dve", lambda e: e.reciprocal(out=st[:, :, 2:3], in_=st[:, :, 1:2]), reads=[B_stall], writes=[B_stall])

        def r_s1(i):
            p = i % NB1
            kk.op("dve", lambda e: e.scalar_tensor_tensor(
                out=tn[p][:], in0=hres[:, i, :], scalar=st[:, i, 2:3], in1=gffn[:], op0=ALU.mult, op1=ALU.mult),
                reads=[B_h[i], B_stall, B_gf], writes=[B_tn[p]])

        def r_s2(i):
            p = i % NB1
            q = i % 2
            for c in range(KC):
                kk.op("pe", lambda e, c=c: e.transpose(ptr[q][:, c, :], tn[p][:, c * 128:(c + 1) * 128], ident_f[:]),
                      reads=[B_tn[p], B_const], writes=[B_ptr[q]])
            kk.op("act", lambda e: e.copy(out=tTf[q][:], in_=ptr[q][:]), reads=[B_ptr[q]], writes=[B_tTf[q]])
            kk.op("pool", lambda e: e.tensor_copy(out=xT[:, :, i * 128:(i + 1) * 128], in_=tTf[q][:]),
                  reads=[B_tTf[q]], writes=[B_xT[i]])
            for c in range(KC):
                kk.op("pe", lambda e, c=c: e.matmul(plg[q][:, 0:36], lhsT=tTf[q][:, c, :], rhs=wr[:, c, :],
                                                    start=(c == 0), stop=(c == KC - 1)),
                      reads=[B_tTf[q], B_const], writes=[B_plg[q]])
            kk.op("dve", lambda e: e.tensor_tensor(out=lg[:, i, :], in0=plg[q][:, 0:36], in1=rbias[:], op=ALU.add),
                  reads=[B_plg[q], B_const], writes=[B_lg])

        for k in range(NT + 1):
            if k < NT:
                r_s1(k)
            if k >= 1:
                r_s2(k - 1)
        rt = sb("rt", [128, NT, 42], F32, ts)
        r4 = sb("r4", [128, NT, 8], F32, ts)
        m8 = sb("m8M", [128, NT, 8], F32, ts)
        B_rt = kk.buf()
        lgg = lg[:, :, 0:4]
        lge = lg[:, :, 4:36].rearrange("p t (g e) -> p t g e", g=4)
        mg, sg, gp, ohg = rt[:, :, 0:1], rt[:, :, 1:2], rt[:, :, 2:3], rt[:, :, 4:8]
        eg = rt[:, :, 8:12]
        ein, ex, selm = rt[:, :, 16:24], rt[:, :, 24:32], rt[:, :, 32:40]
        den, fc_ = rt[:, :, 40:41], rt[:, :, 41:42]
        R = dict(reads=[B_lg, B_rt], writes=[B_rt])
        kk.op("dve", lambda e: e.tensor_reduce(out=mg, in_=lgg, axis=AX.X, op=ALU.max), **R)
        kk.op("dve", lambda e: e.tensor_tensor(out=eg, in0=lgg, in1=mg.to_broadcast([128, NT, 4]), op=ALU.subtract), **R)
        kk.op("act", lambda e: e.activation(out=eg, in_=eg, func=AF.Exp), **R)
        kk.op("dve", lambda e: e.reduce_sum(out=sg, in_=eg, axis=AX.X), **R)
        kk.op("dve", lambda e: e.reciprocal(out=gp, in_=sg), **R)
        kk.op("dve", lambda e: e.tensor_tensor(out=ohg, in0=lgg, in1=mg.to_broadcast([128, NT, 4]), op=ALU.is_equal), **R)
        kk.op("dve", lambda e: e.tensor_tensor(out=ein, in0=lge[:, :, 0, :], in1=ohg[:, :, 0:1].to_broadcast([128, NT, 8]), op=ALU.mult), **R)
        for g_ in range(1, 4):
            kk.op("dve", lambda e, g_=g_: e.tensor_tensor(out=r4[:], in0=lge[:, :, g_, :],
                                                        in1=ohg[:, :, g_:g_ + 1].to_broadcast([128, NT, 8]), op=ALU.mult), **R)
            kk.op("dve", lambda e: e.tensor_tensor(out=ein, in0=ein, in1=r4[:], op=ALU.add), **R)
        for tb in range(NT):
            kk.op("dve", lambda e, tb=tb: e.max(out=m8[:, tb, :], in_=rt[:, tb, 16:24]), **R)
        kk.op("dve", lambda e: e.tensor_tensor(out=ex, in0=ein, in1=m8[:, :, 0:1].to_broadcast([128, NT, 8]), op=ALU.subtract), **R)
        kk.op("act", lambda e: e.activation(out=ex, in_=ex, func=AF.Exp), **R)
        kk.op("dve", lambda e: e.tensor_tensor(out=selm, in0=ein, in1=m8[:, :, 1:2].to_broadcast([128, NT, 8]), op=ALU.is_ge), **R)
        kk.op("dve", lambda e: e.tensor_tensor(out=ex, in0=ex, in1=selm, op=ALU.mult), **R)
        kk.op("dve", lambda e: e.reduce_sum(out=den, in_=ex, axis=AX.X), **R)
        kk.op("dve", lambda e: e.reciprocal(out=fc_, in_=den), **R)
        kk.op("dve", lambda e: e.tensor_tensor(out=fc_, in0=fc_, in1=gp, op=ALU.mult), **R)
        kk.op("dve", lambda e: e.tensor_tensor(out=ex, in0=ex, in1=fc_.to_broadcast([128, NT, 8]), op=ALU.mult), **R)
        kk.op("dve", lambda e: e.tensor_tensor(
            out=comb[:].rearrange("p t (g e) -> p t g e", g=4), in0=ohg.unsqueeze(3).to_broadcast([128, NT, 4, 8]),
            in1=ex.unsqueeze(2).to_broadcast([128, NT, 4, 8]), op=ALU.mult), reads=[B_rt], writes=[B_comb])
        kk.barrier()
    if stage == 7:
        dump("comb", comb[:], [128, NT, 32], [B_comb])
        dump("tT", xT[:], [128, KC, S], B_xT, BF16)
        return

    with ExitStack() as ts:
        wgu.append(sb("wgu1", [128, 2, KC, 512], BF16, ts))
        wdn.append(sb("wdn1", [128, 2, 2, D], BF16, ts))
        pgu = [psum(f"pgu{i}", [128, 512], F32, ts) for i in range(2)]
        ptrE = [psum(f"ptrE{i}", [128, 8, 128], BF16, ts) for i in range(2)]
        py = [psum(f"pyE{i}", [128, D], F32, ts) for i in range(2)]
        B_pgu = [kk.pbuf() for _ in range(2)]
        B_ptr = [kk.pbuf() for _ in range(2)]
        B_py = [kk.pbuf() for _ in range(2)]
        sgs = [sb(f"sgs{i}", [128, 256], F32, ts) for i in range(3)]
        hh = [sb(f"hh{i}", [128, 256], BF16, ts) for i in range(3)]
        hT = [sb(f"hT{i}", [128, 2, 128], BF16, ts) for i in range(4)]
        B_sgs = [kk.buf() for _ in range(3)]
        B_hh = [kk.buf() for _ in range(3)]
        B_hT = [kk.buf() for _ in range(4)]

        NPAIR = L['nexp'] // 2
        units = [(pr, i, ei) for pr in range(NPAIR) for i in range(NT) for ei in range(2)]
        if NPAIR > 1:
            load_pair(1)

        def m_A(k):
            pr, i, ei = units[k]
            w = pr % NWB
            e_ = 2 * pr + ei
            p = k % 3
            pb = k % 2
            for c in range(KC):
                kk.op("pe", lambda e, c=c: e.matmul(
                    pgu[pb][:, :], lhsT=xT[:, c, i * 128:(i + 1) * 128], rhs=wgu[w][:, ei, c, :],
                    start=(c == 0), stop=(c == KC - 1)), reads=[B_xT[i], B_wgu[w]], writes=[B_pgu[pb]])
            kk.op("act", lambda e: e.activation(out=sgs[p][:], in_=pgu[pb][:, 0:256], func=AF.Silu),
                  reads=[B_pgu[pb]], writes=[B_sgs[p]])
            kk.op("dve", lambda e: e.scalar_tensor_tensor(
                out=hh[p][:], in0=pgu[pb][:, 256:512], scalar=comb[:, i, e_:e_ + 1], in1=sgs[p][:],
                op0=ALU.mult, op1=ALU.mult), reads=[B_pgu[pb], B_sgs[p], B_comb], writes=[B_hh[p]])

        def m_B(k):
            p = k % 3
            s_ = k % 4
            tb_ = k % 2
            for fc in range(2):
                kk.op("pe", lambda e, fc=fc: e.transpose(ptrE[tb_][:, fc, :], hh[p][:, fc * 128:(fc + 1) * 128], ident_b[:]),
                      reads=[B_hh[p], B_const], writes=[B_ptr[tb_]])
            kk.op("act", lambda e: e.copy(out=hT[s_][:], in_=ptrE[tb_][:, 0:2, :]),
                  reads=[B_ptr[tb_]], writes=[B_hT[s_]])

        def m_C(k):
            pr, i, ei = units[k]
            w = pr % NWB
            yp = (k // 2) % 2
            slots = [(k - 1) % 4, k % 4]
            for e2 in range(2):
                for fc in range(2):
                    for hf in range(2):
                        kk.op("pe", lambda e, e2=e2, fc=fc, hf=hf: e.matmul(
                            py[yp][:, hf * 512:(hf + 1) * 512], lhsT=hT[slots[e2]][:, fc, :],
                            rhs=wdn[w][:, e2, fc, hf * 512:(hf + 1) * 512],
                            start=(e2 == 0 and fc == 0), stop=(e2 == 1 and fc == 1)),
                            reads=[B_hT[slots[e2]], B_wdn[w]], writes=[B_py[yp]])
            kk.op("dve", lambda e: e.tensor_tensor(out=hres[:, i, :], in0=hres[:, i, :], in1=py[yp][:], op=ALU.add),
                  reads=[B_py[yp], B_h[i]], writes=[B_h[i]])

        nu = len(units)
        for k in range(nu + 2):
            if k < nu:
                m_A(k)
            if 0 <= k - 1 < nu:
                m_B(k - 1)
            if 0 <= k - 2 < nu and units[k - 2][2] == 1:
                m_C(k - 2)
                pr_, i_, _ = units[k - 2]
                if i_ == NT - 1 and pr_ + 2 < NPAIR:
                    load_pair(pr_ + 2)
        kk.barrier()

    with ExitStack() as ts:
        gfin = sb("gfin", [128, D], F32, ts)
        B_gfi = kk.buf()
        kk.dma("sp", gfin[:], bcast_rows(lnfin_d, D), writes=[B_gfi])
        NOB = 3
        ob = [sb(f"ob{i}", [128, D], F32, ts) for i in range(NOB)]
        junk = [sb(f"junkF{i}", [128, D], BF16, ts) for i in range(2)]
        st = sb("stF", [128, 3, NT], F32, ts)
        B_ob = [kk.buf() for _ in range(NOB)]
        B_st = kk.buf()
        B_junk = [kk.buf() for _ in range(2)]
        B_sth = [kk.buf() for _ in range(2)]
        HN = NT // 2
        for hf in range(2):
            lo, hi = hf * HN, (hf + 1) * HN
            for i in range(lo, hi):
                kk.op("act", lambda e, i=i: e.activation(out=junk[i % 2][:], in_=hres[:, i, :], func=AF.Square,
                                                        accum_out=st[:, 0, i:i + 1]),
                      reads=[B_h[i]], writes=[B_sth[hf], B_junk[i % 2]])
            kk.op("act", lambda e, lo=lo, hi=hi: e.activation(out=st[:, 1, lo:hi], in_=st[:, 0, lo:hi], func=AF.Sqrt,
                                                           bias=EPS, scale=1.0 / D),
                  reads=[B_sth[hf]], writes=[B_sth[hf]])
            kk.op("dve", lambda e, lo=lo, hi=hi: e.reciprocal(out=st[:, 2, lo:hi], in_=st[:, 1, lo:hi]),
                  reads=[B_sth[hf]], writes=[B_sth[hf]])
        for i in range(NT):
            p = i % NOB
            hf = i // HN
            kk.op("dve", lambda e, p=p, i=i: e.scalar_tensor_tensor(
                out=ob[p][:], in0=hres[:, i, :], scalar=st[:, 2, i:i + 1], in1=gfin[:], op0=ALU.mult, op1=ALU.mult),
                reads=[B_h[i], B_sth[hf], B_gfi], writes=[B_ob[p]])
            kk.dma("sp", out_d.ap()[sq, i * 128:(i + 1) * 128, :], ob[p][:], reads=[B_ob[p]], is_output=True)
        kk.barrier()


_CACHE = {}


def _prep_inputs(inputs, core, nseq):
    m = {}
    m["x"] = np.ascontiguousarray(inputs["x"][core * nseq:(core + 1) * nseq])
    m["rel_bias"] = np.ascontiguousarray(inputs["rel_bias"])
    for k_ in ("ln_mix", "w_in", "cmp_pos_k", "cmp_pos_v", "cmp_k_w1", "cmp_k_w2", "cmp_v_w1", "cmp_v_w2",
               "diff_lq1", "diff_lk1", "diff_lq2", "diff_lk2", "diff_subln", "w_out", "ln_ffn",
               "router_group_w", "router_group_b", "router_expert_w", "router_expert_b",
               "exp_w_gate", "exp_w_up", "exp_w_down"):
        a = np.asarray(inputs[k_])
        if k_ in ("ln_mix", "diff_lq1", "diff_lk1", "diff_lq2", "diff_lk2", "diff_subln", "ln_ffn",
                  "router_group_b", "router_expert_b"):
            m[k_] = np.ascontiguousarray(a.reshape(1, -1))
        else:
            m[k_] = np.ascontiguousarray(a[0])
    m["ln_final"] = np.ascontiguousarray(np.asarray(inputs["ln_final"]).reshape(1, -1))
    return m


def kernel(**inputs):
    inputs = {k_: np.asarray(v_) for k_, v_ in inputs.items()}
    n_cores = 8
    nseq = inputs["x"].shape[0] // n_cores
    if "nc" not in _CACHE:
        _CACHE["nc"] = build_nc(nseq=nseq)[0]
        _CACHE["hc"] = _host_consts()
    nc = _CACHE["nc"]
    hc = _CACHE["hc"]
    shared = _prep_inputs(inputs, 0, nseq)
    in_maps = []
    for c in range(n_cores):
        m = dict(shared)
        m["x"] = np.ascontiguousarray(inputs["x"][c * nseq:(c + 1) * nseq])
        for k_, v_ in hc.items():
            m["hc_" + k_] = v_
        in_maps.append(m)
    res = run_bass_kernel_spmd(nc, in_maps, core_ids=list(range(n_cores)))
    out = np.concatenate([np.asarray(r["out"]) for r in res.results], axis=0)
    return out.astype(np.float32)
```

```python
import math
from contextlib import ExitStack

import numpy as np
import ml_dtypes

import concourse.bass as bass
import concourse.mybir as mybir
from concourse.bass_utils import run_bass_kernel_spmd

F32 = mybir.dt.float32
BF16 = mybir.dt.bfloat16
AF = mybir.ActivationFunctionType
ALU = mybir.AluOpType
AX = mybir.AxisListType

S = 2048
D = 1024
NT = 16
KC = 8
D_IN = 2840
NEGB = -30000.0
EPS = 1e-6
SC_A = 0.125
SC_B = 32 ** -0.5
LAMBDA_INIT = 0.8 - 0.6 * math.exp(-0.3 * 0)
TINY = 1e-30

C_QA, C_KC, C_VC, C_KS, C_VS, C_KW, C_VW, C_GT, C_QB, C_KB, C_VB = (
    0, 512, 640, 768, 896, 1024, 1152, 1280, 1304, 1816, 2328)

SELF_RAW = True
STRICT_SELF = True


def _bucket(n):
    n = np.maximum(n, 0)
    nf = np.maximum(n, 1).astype(np.float32)
    large = 16 + (np.log(nf / np.float32(16)) / np.float32(math.log(128 / 16)) * np.float32(16)).astype(np.int32)
    large = np.minimum(large, 31)
    return np.where(n < 16, n, large)


def _host_consts():
    c = {}
    c["ident"] = np.eye(128, dtype=np.float32)
    k = np.arange(128)[:, None]
    t = np.arange(128)[None, :]
    m_ = np.arange(384)[None, :] - 127
    bk1 = _bucket(m_)
    c["oh1"] = ((bk1 == np.arange(32)[:, None]) & (m_ >= 0)).astype(np.float32)
    c["mask1"] = np.broadcast_to(np.where(m_ >= 0, 0.0, NEGB).astype(np.float32), (128, 384)).copy()
    cm = np.eye(32, dtype=np.float32)
    cm[31, :] -= 1.0
    c["cmat"] = cm
    c["mask4"] = np.where(t < k, 0.0, NEGB).astype(np.float32)
    j = np.arange(247)[None, :]
    tt = np.arange(128)[:, None]
    dc = tt - 16 * (j - 120) - 31
    bkc = _bucket(dc)
    ohc = np.zeros((128, 31, 247), np.float32)
    for b in range(31):
        ohc[:, b, :] = ((bkc == b) & (dc >= 0))
    c["ohc"] = ohc.astype(ml_dtypes.bfloat16)
    c["maskc"] = np.where(dc >= 0, 0.0, NEGB).astype(np.float32)
    starts = np.arange(127) * 16
    sel_start = np.arange(32) * 64
    ovl = ((starts[:, None] <= sel_start[None, :] + 63) & (starts[:, None] + 31 >= sel_start[None, :]))
    c["ovl"] = ovl.astype(np.float32)
    tpos = (np.arange(16)[None, :, None] * 128 + np.arange(128)[:, None, None])
    cur = tpos // 64
    blk = np.arange(32)[None, None, :]
    valid = blk <= cur
    forced = (blk == 0) | ((cur - blk >= 0) & (cur - blk < 2))
    c["nfv"] = (valid & ~forced).astype(np.float32)
    c["addc"] = np.where(valid & forced, 1e9, np.where(valid, 0.0, -1e9)).astype(np.float32)
    c["valid"] = valid.astype(np.float32)
    e = (np.arange(2048)[None, :] // 64 == np.arange(32)[:, None])
    c["eexp"] = e.astype(np.float32)
    return c


class Buf:
    __slots__ = ("name", "w", "r", "dw", "dr", "excl")

    def __init__(self, name, excl=False):
        self.name = name
        self.excl = excl
        self.w = {}
        self.r = {}
        self.dw = []
        self.dr = []


class Eng:
    def __init__(self, name, h, sem):
        self.name = name
        self.h = h
        self.sem = sem
        self.cnt = 0
        self.known = {}


class K:
    def __init__(self, nc, es):
        self.nc = nc
        self.es = es
        self.eng = {}
        for name, h in (("pe", nc.tensor), ("act", nc.scalar), ("dve", nc.vector), ("pool", nc.gpsimd),
                        ("sp", nc.sync)):
            sem = es.enter_context(nc.semaphore("sem_" + name))
            self.eng[name] = Eng(name, h, sem)
        self.ndsem = 20
        self.dsem = {}
        self.dval = {}
        self.drr = {}
        for q in ("sp", "pool"):
            self.dsem[q] = [es.enter_context(nc.semaphore(f"dsem_{q}{i}")) for i in range(self.ndsem)]
            self.dval[q] = [0] * self.ndsem
            self.dbar = getattr(self, "dbar", {})
            self.dbar[q] = [0] * self.ndsem
            self.drr[q] = 0
        self.nbuf = 0
        self.out_dmas = []

    def buf(self, name=None, excl=False):
        self.nbuf += 1
        return Buf(name or f"b{self.nbuf}", excl)

    def pbuf(self, name=None):
        return self.buf(name, excl=True)

    def _semof(self, key):
        if key[0] == "e":
            return self.eng[key[1]].sem
        return self.dsem[key[1]][key[2]]

    def _collect(self, eng, reads, writes):
        waits = {}

        def need(key, val):
            if val > waits.get(key, 0):
                waits[key] = val
        for b in reads:
            for en, idx in b.w.items():
                if en == eng and (eng == "pe" or not SELF_RAW):
                    continue
                need(("e", en), idx)
            for key, v in b.dw:
                need(key, v)
            if b.excl:
                for en, idx in b.r.items():
                    if en != eng:
                        need(("e", en), idx)
        for b in writes:
            for en, idx in b.w.items():
                if en == eng and (eng == "pe" or not STRICT_SELF):
                    continue
                need(("e", en), idx)
            for en, idx in b.r.items():
                if en == eng and (eng == "pe" or not STRICT_SELF):
                    continue
                need(("e", en), idx)
            for key, v in b.dw:
                need(key, v)
            for key, v in b.dr:
                need(key, v)
        return waits

    def _emit_waits(self, E, waits):
        for key, val in waits.items():
            if E.known.get(key, 0) < val:
                E.h.wait_ge(self._semof(key), val)
                E.known[key] = val

    def op(self, eng, fn, reads=(), writes=()):
        E = self.eng[eng]
        self._emit_waits(E, self._collect(eng, reads, writes))
        ins = fn(E.h)
        E.cnt += 1
        ins.then_inc(E.sem, 1)
        for b in reads:
            b.r[eng] = E.cnt
        for b in writes:
            b.w = {eng: E.cnt}
            b.r = {}
            b.dw = []
            b.dr = []
        return E.cnt

    def dma(self, q, out_ap, in_ap, reads=(), writes=(), is_output=False, nobarrier=False):
        E = self.eng[q]
        self._emit_waits(E, self._collect(q, reads, writes))
        i = self.drr[q]
        self.drr[q] = (i + 1) % self.ndsem
        key = ("d", q, i)
        if E.known.get(key, 0) < self.dval[q][i]:
            E.h.wait_ge(self.dsem[q][i], self.dval[q][i])
            E.known[key] = self.dval[q][i]
        self.dval[q][i] += 16
        val = self.dval[q][i]
        if not nobarrier:
            self.dbar[q][i] = val
        E.h.dma_start(out=out_ap, in_=in_ap).then_inc(self.dsem[q][i], 16)
        for b in reads:
            b.dr.append((key, val))
            if len(b.dr) > 8:
                b.dr = b.dr[-8:]
        for b in writes:
            if b.r or b.w or b.dr:
                b.dw = []
            b.dw.append((key, val))
            b.w = {}
            b.r = {}
            b.dr = []
        if is_output:
            self.out_dmas.append((key, val))

    def defer(self, eng, fn, reads=(), writes=(), tag=None):
        if not hasattr(self, "pending"):
            self.pending = []
        self.pending.append((eng, fn, list(reads), list(writes), tag))

    def drain(self, n=None, until_tag=None):
        pend = getattr(self, "pending", [])
        k = 0
        while pend:
            if until_tag is not None and not any(t[4] == until_tag for t in pend):
                break
            if until_tag is None and n is not None and k >= n:
                break
            eng, fn, r, w, _ = pend.pop(0)
            self.op(eng, fn, reads=r, writes=w)
            k += 1

    def barrier(self):
        finals = {("e", n): self.eng[n].cnt for n in ("pe", "act", "dve", "pool")}
        for q in ("sp", "pool"):
            for i in range(self.ndsem):
                finals[("d", q, i)] = self.dbar[q][i]
        for n in ("pe", "act", "dve", "pool", "sp"):
            E = self.eng[n]
            w = {k: v for k, v in finals.items() if v > 0 and not (k[0] == "e" and k[1] == n)}
            self._emit_waits(E, w)


def build_nc(nseq=2, stage=99, dbg=False, nexp=32):
    nc = bass.Bass("TRN2", target_bir_lowering=False)
    hc = _host_consts()

    def din(name, shape, dt=F32):
        return nc.dram_tensor(name, list(shape), dt, kind="ExternalInput")

    x_d = din("x", [nseq, S, D])
    relb_d = din("rel_bias", [32, 16])
    lnmix_d = din("ln_mix", [1, D])
    win_d = din("w_in", [D, D_IN])
    posk_d = din("cmp_pos_k", [32, 64])
    posv_d = din("cmp_pos_v", [32, 64])
    ck1_d = din("cmp_k_w1", [2048, 128])
    ck2_d = din("cmp_k_w2", [128, 64])
    cv1_d = din("cmp_v_w1", [2048, 128])
    cv2_d = din("cmp_v_w2", [128, 64])
    lq1_d = din("diff_lq1", [1, 32])
    lk1_d = din("diff_lk1", [1, 32])
    lq2_d = din("diff_lq2", [1, 32])
    lk2_d = din("diff_lk2", [1, 32])
    subln_d = din("diff_subln", [1, 64])
    wout_d = din("w_out", [D, D])
    lnffn_d = din("ln_ffn", [1, D])
    rgw_d = din("router_group_w", [D, 4])
    rgb_d = din("router_group_b", [1, 4])
    rew_d = din("router_expert_w", [D, 32])
    reb_d = din("router_expert_b", [1, 32])
    wg_d = din("exp_w_gate", [nexp, D, 256])
    wu_d = din("exp_w_up", [nexp, D, 256])
    wd_d = din("exp_w_down", [nexp, 256, D])
    lnfin_d = din("ln_final", [1, D])
    hcd = {}
    for k_, v_ in hc.items():
        hcd[k_] = din("hc_" + k_, v_.shape, BF16 if v_.dtype == ml_dtypes.bfloat16 else F32)
    out_d = nc.dram_tensor("out", [nseq, S, D], F32, kind="ExternalOutput")
    scr_d = nc.dram_tensor("scr_toeplitz", [16, 128, 384], F32)
    dbg_out = {}

    es = ExitStack()
    with es:
        kk = K(nc, es)
        es.enter_context(nc.Block())

        uid = [0]

        def sb(name, shape, dt, stack=es):
            uid[0] += 1
            return stack.enter_context(nc.sbuf_tensor(f"{name}_{uid[0]}", list(shape), dt))

        def psum(name, shape, dt, stack=es):
            uid[0] += 1
            return stack.enter_context(nc.psum_tensor(f"{name}_{uid[0]}", list(shape), dt))

        def bcast_rows(dt_, n):
            return bass.AP(dt_, 0, [[0, 128], [1, n]])

        def dump(name, ap_sb, shape, bufs, dt=F32):
            if not dbg:
                return
            t = nc.dram_tensor("dbg_" + name, list(shape), dt, kind="ExternalOutput")
            dbg_out[name] = t
            kk.dma("sp", t.ap(), ap_sb, reads=bufs, is_output=True)

        ident_f = sb("ident_f", [128, 128], F32)
        ident_b = sb("ident_b", [128, 128], BF16)
        B_const = kk.buf("const")
        kk.dma("sp", ident_f[:], hcd["ident"].ap(), writes=[B_const])
        kk.dma("pool", ident_b[:], hcd["ident"].ap(), writes=[B_const])
        mask4 = sb("mask4", [128, 128], F32)
        kk.dma("sp", mask4[:], hcd["mask4"].ap(), writes=[B_const])
        ovl = sb("ovl", [127, 32], F32)
        kk.dma("sp", ovl[:], hcd["ovl"].ap(), writes=[B_const])
        subln = sb("subln", [128, 64], F32)
        kk.dma("sp", subln[:], bcast_rows(subln_d, 64), writes=[B_const])
        rbias = sb("rbias", [128, 36], F32)
        kk.dma("sp", rbias[:, 0:4], bcast_rows(rgb_d, 4), writes=[B_const])
        kk.dma("sp", rbias[:, 4:36], bcast_rows(reb_d, 32), writes=[B_const])
        wr = sb("wr", [128, KC, 36], F32)
        kk.dma("sp", wr[:, :, 0:4], rgw_d.ap().rearrange("(kc p) c -> p kc c", p=128), writes=[B_const])
        kk.dma("sp", wr[:, :, 4:36], rew_d.ap().rearrange("(kc p) c -> p kc c", p=128), writes=[B_const])
        w2k = sb("w2k", [128, 64], BF16)
        w2v = sb("w2v", [128, 64], BF16)
        B_cw = kk.buf("cw")
        kk.dma("pool", w2k[:], ck2_d.ap(), writes=[B_cw])
        kk.dma("pool", w2v[:], cv2_d.ap(), writes=[B_cw])

        W1 = sb("W1", [128, KC, 1536], BF16)
        B_W1 = kk.buf("W1")
        win_v = win_d.ap().rearrange("(kc p) c -> p kc c", p=128)
        wout_v = wout_d.ap().rearrange("(kc p) c -> p kc c", p=128)

        def load_W1(which):
            if which == "A":
                for c0 in range(0, 1304, 326):
                    kk.dma("pool", W1[:, :, c0:c0 + 326], win_v[:, :, c0:c0 + 326], writes=[B_W1], nobarrier=True)
            elif which == "B":
                for c0 in range(0, 1536, 384):
                    kk.dma("pool", W1[:, :, c0:c0 + 384], win_v[:, :, C_QB + c0:C_QB + c0 + 384], writes=[B_W1], nobarrier=True)
            else:
                for c0 in range(0, D, 256):
                    kk.dma("pool", W1[:, :, c0:c0 + 256], wout_v[:, :, c0:c0 + 256], writes=[B_W1], nobarrier=True)

        lqk = sb("lqk", [128, 4, 32], F32)
        B_l = kk.buf("lam")
        for i_, t_ in enumerate((lq1_d, lk1_d, lq2_d, lk2_d)):
            kk.dma("sp", lqk[:, i_, :], bcast_rows(t_, 32), writes=[B_l])
        lam_t = sb("lam_t", [128, 8], F32)
        neglam = sb("neglam", [128, 1], F32)
        kk.op("dve", lambda e: e.tensor_tensor(out=lqk[:, 0, :], in0=lqk[:, 0, :], in1=lqk[:, 1, :], op=ALU.mult),
              reads=[B_l], writes=[B_l])
        kk.op("dve", lambda e: e.tensor_tensor(out=lqk[:, 2, :], in0=lqk[:, 2, :], in1=lqk[:, 3, :], op=ALU.mult),
              reads=[B_l], writes=[B_l])
        kk.op("dve", lambda e: e.reduce_sum(out=lam_t[:, 0:1], in_=lqk[:, 0, :], axis=AX.X), reads=[B_l], writes=[B_l])
        kk.op("dve", lambda e: e.reduce_sum(out=lam_t[:, 1:2], in_=lqk[:, 2, :], axis=AX.X), reads=[B_l], writes=[B_l])
        kk.op("act", lambda e: e.activation(out=lam_t[:, 2:4], in_=lam_t[:, 0:2], func=AF.Exp), reads=[B_l], writes=[B_l])
        kk.op("dve", lambda e: e.tensor_tensor(out=lam_t[:, 4:5], in0=lam_t[:, 3:4], in1=lam_t[:, 2:3], op=ALU.subtract),
              reads=[B_l], writes=[B_l])
        kk.op("dve", lambda e: e.tensor_scalar_add(out=neglam[:], in0=lam_t[:, 4:5], scalar1=-LAMBDA_INIT),
              reads=[B_l], writes=[B_l])

        TN = sb("TN", [128, 2, 2, 4, 128], F32)
        TD = sb("TD", [128, 8, 256], F32)
        TC = sb("TC", [128, 2, 4, 247], F32)
        B_TN = kk.buf("TN")
        B_TD = kk.buf("TD")
        B_TC = kk.buf("TC")
        with ExitStack() as ts:
            tab = sb("tab", [128, 32, 16], F32, ts)
            val = sb("val", [128, 32, 16], F32, ts)
            ohc = sb("ohc", [128, 31, 247], BF16, ts)
            maskc = sb("maskc", [128, 247], F32, ts)
            B_t = kk.buf("tab")
            B_oh = kk.buf("oh")
            kk.dma("sp", tab[:].rearrange("p b h -> p (b h)"), bass.AP(relb_d, 0, [[0, 128], [1, 512]]), writes=[B_t])
            kk.dma("sp", ohc[:], hcd["ohc"].ap(), writes=[B_oh])
            kk.dma("sp", maskc[:], hcd["maskc"].ap(), writes=[B_oh])
            kk.op("dve", lambda e: e.tensor_tensor(out=val[:], in0=tab[:], in1=tab[:, 31:32, :].to_broadcast([128, 32, 16]),
                                                   op=ALU.subtract), reads=[B_t], writes=[B_t])
            kk.op("dve", lambda e: e.tensor_scalar_mul(out=val[:, :, 0:8], in0=val[:, :, 0:8], scalar1=1.0 / SC_A),
                  reads=[B_t], writes=[B_t])
            kk.op("dve", lambda e: e.tensor_scalar_mul(out=val[:, :, 8:16], in0=val[:, :, 8:16], scalar1=1.0 / SC_B),
                  reads=[B_t], writes=[B_t])
            cmat = sb("cmat", [32, 32], F32, ts)
            oh1 = sb("oh1", [32, 384], F32, ts)
            mask1 = sb("mask1", [128, 384], F32, ts)
            tab32 = sb("tab32", [32, 16], F32, ts)
            vs32 = sb("vs32", [32, 16], F32, ts)
            valb = sb("valb", [32, 16, 128], F32, ts)
            frep = sb("frep", [128, 16, 384], F32, ts)
            pv32 = psum("pv32", [128, 512], F32, ts)
            pF = [psum(f"pF{i}", [128, 512], F32, ts) for i in range(2)]
            B_sk = kk.buf()
            B_pv = kk.pbuf()
            B_pF = [kk.pbuf() for _ in range(2)]
            B_fr = kk.buf()
            kk.dma("sp", cmat[:], hcd["cmat"].ap(), writes=[B_sk])
            kk.dma("sp", oh1[:], hcd["oh1"].ap(), writes=[B_sk])
            kk.dma("sp", mask1[:], hcd["mask1"].ap(), writes=[B_sk])
            kk.dma("sp", tab32[:], relb_d.ap(), writes=[B_sk])
            kk.op("pe", lambda e: e.matmul(pv32[0:32, 0:16], lhsT=cmat[:], rhs=tab32[:], start=True, stop=True),
                  reads=[B_sk], writes=[B_pv])
            kk.op("dve", lambda e: e.tensor_scalar_mul(out=vs32[:, 0:8], in0=pv32[0:32, 0:8], scalar1=1.0 / SC_A),
                  reads=[B_pv], writes=[B_sk])
            kk.op("dve", lambda e: e.tensor_scalar_mul(out=vs32[:, 8:16], in0=pv32[0:32, 8:16], scalar1=1.0 / SC_B),
                  reads=[B_pv], writes=[B_sk])
            kk.op("dve", lambda e: e.tensor_copy(out=valb[:], in_=vs32[:].unsqueeze(2).to_broadcast([32, 16, 128])),
                  reads=[B_sk], writes=[B_sk])
            for h in range(16):
                p = h % 2
                kk.op("pe", lambda e, p=p, h=h: e.matmul(pF[p][:, 0:384], lhsT=valb[:, h, :], rhs=oh1[:], start=True, stop=True),
                      reads=[B_sk], writes=[B_pF[p]])
                kk.op("dve", lambda e, p=p, h=h: e.tensor_tensor(out=frep[:, h, :], in0=pF[p][:, 0:384], in1=mask1[:], op=ALU.add),
                      reads=[B_pF[p], B_sk], writes=[B_fr])
            B_scr = kk.buf()
            kk.dma("sp", scr_d.ap().rearrange("h p m -> p h m"), frep[:], reads=[B_fr], writes=[B_scr])
            for g in range(2):
                for r in range(4):
                    hh = 4 * g + r
                    kk.dma("sp", TN[:, g, :, r, :], bass.AP(scr_d, hh * 128 * 384 + 127, [[383, 128], [128, 2], [1, 128]]),
                           reads=[B_scr], writes=[B_TN])
            for h in range(8):
                kk.dma("sp", TD[:, h, :], bass.AP(scr_d, (8 + h) * 128 * 384 + 127, [[383, 128], [1, 256]]),
                       reads=[B_scr], writes=[B_TD])
            accs = []
            for g in range(2):
                for r in range(4):
                    bC = kk.buf()
                    engc = "dve"
                    kk.op(engc, lambda e, g=g, r=r: e.tensor_copy(out=TC[:, g, r, :], in_=maskc[:]),
                          reads=[B_oh], writes=[bC])
                    accs.append((engc, TC[:, g, r, :], lambda b: ohc[:, b, :], 4 * g + r, bC))
            ptmps = [sb(f"ptmp{i}", [128, 256], F32, ts) for i in range(4)]
            B_ptmps = [kk.buf() for _ in range(4)]
            pi = 0
            for b in range(31):
                for (eng, oap, ohf, col, bb) in accs:
                    if eng == "dve":
                        kk.op("dve", lambda e, oap=oap, ohf=ohf, col=col, b=b: e.scalar_tensor_tensor(
                            out=oap, in0=ohf(b), scalar=val[:, b, col:col + 1], in1=oap, op0=ALU.mult, op1=ALU.add),
                            reads=[B_oh, B_t, bb], writes=[bb])
                    else:
                        q_ = pi % 4
                        pi += 1
                        kk.op("pool", lambda e, ohf=ohf, col=col, b=b, q_=q_: e.tensor_scalar_mul(
                            out=ptmps[q_][:, 0:247], in0=ohf(b), scalar1=val[:, b, col:col + 1]),
                            reads=[B_oh, B_t], writes=[B_ptmps[q_]])
                        kk.op("pool", lambda e, oap=oap, q_=q_: e.tensor_tensor(
                            out=oap, in0=oap, in1=ptmps[q_][:, 0:247], op=ALU.add),
                            reads=[B_ptmps[q_], bb], writes=[bb])
            kk.barrier()
        if stage == 0:
            dump("TN", TN[:], [128, 2, 2, 4, 128], [B_TN])
            dump("TD", TD[:], [128, 8, 256], [B_TD])
            dump("TC", TC[:], [128, 2, 4, 247], [B_TC])
            dump("neglam", neglam[:], [128, 1], [B_l])

        if stage > 0:
            load_W1("A")
        for sq in range(nseq if stage > 0 else 0):
            with ExitStack() as ss:
                xT = sb("xT", [128, KC, S], BF16, ss)
                mx = ss.enter_context(ExitStack())
                omix = sb("omix", [128, NT, D], BF16, mx)
                gates = sb("gates", [128, NT, 24], F32, mx)
                B_xT = [kk.buf(f"xT{i}") for i in range(NT)]
                B_om = [kk.buf(f"om{i}") for i in range(NT)]
                B_gates = kk.buf("gates")

                with ExitStack() as ts:
                    gmix = sb("gmix", [128, D], F32, ts)
                    B_gm = kk.buf()
                    kk.dma("sp", gmix[:], bcast_rows(lnmix_d, D), writes=[B_gm])
                    NB0 = 3
                    xs = [sb(f"xs{i}", [128, D], F32, ts) for i in range(NB0)]
                    xn = [sb(f"xn{i}", [128, D], BF16, ts) for i in range(NB0)]
                    junk = sb("junkA", [128, D], BF16, ts)
                    st = sb("stA", [128, NT, 4], F32, ts)
                    ptr = [psum(f"ptrA{i}", [128, KC, 128], BF16, ts) for i in range(2)]
                    B_xs = [kk.buf() for _ in range(NB0)]
                    B_xn = [kk.buf() for _ in range(NB0)]
                    B_ptr = [kk.pbuf() for _ in range(2)]
                    B_st = [kk.buf() for _ in range(NT)]
                    B_junk = kk.buf()

                    def a0_s1(i):
                        p = i % NB0
                        kk.dma("sp", xs[p][:], x_d.ap()[sq, i * 128:(i + 1) * 128, :], writes=[B_xs[p]])
                        kk.op("act", lambda e: e.activation(out=junk[:], in_=xs[p][:], func=AF.Square,
                                                            accum_out=st[:, i, 0:1]),
                              reads=[B_xs[p]], writes=[B_st[i], B_junk])
                        kk.op("act", lambda e: e.activation(out=st[:, i, 1:2], in_=st[:, i, 0:1], func=AF.Sqrt,
                                                            bias=EPS, scale=1.0 / D), reads=[B_st[i]], writes=[B_st[i]])
                        kk.op("dve", lambda e: e.reciprocal(out=st[:, i, 2:3], in_=st[:, i, 1:2]),
                              reads=[B_st[i]], writes=[B_st[i]])
                        kk.op("dve", lambda e: e.scalar_tensor_tensor(
                            out=xn[p][:], in0=xs[p][:], scalar=st[:, i, 2:3], in1=gmix[:], op0=ALU.mult, op1=ALU.mult),
                            reads=[B_xs[p], B_st[i], B_gm], writes=[B_xn[p]])

                    def a0_s2(i):
                        p = i % NB0
                        pp_ = i % 2
                        for c in range(KC):
                            kk.op("pe", lambda e, c=c: e.transpose(ptr[pp_][:, c, :], xn[p][:, c * 128:(c + 1) * 128],
                                                                   ident_b[:]),
                                  reads=[B_xn[p], B_const], writes=[B_ptr[pp_]])
                        if i % 2 == 0:
                            kk.op("act", lambda e: e.copy(out=xT[:, :, i * 128:(i + 1) * 128], in_=ptr[pp_][:]),
                                  reads=[B_ptr[pp_]], writes=[B_xT[i]])
                        else:
                            kk.op("dve", lambda e: e.tensor_copy(out=xT[:, :, i * 128:(i + 1) * 128], in_=ptr[pp_][:]),
                                  reads=[B_ptr[pp_]], writes=[B_xT[i]])

                    for k in range(NT + 1):
                        if k < NT:
                            a0_s1(k)
                        if k >= 1:
                            a0_s2(k - 1)
                    kk.barrier()
                if stage == 1:
                    dump("xT", xT[:], [128, KC, S], B_xT, BF16)
                    continue

                with ExitStack() as ns:
                    build_nsa(nc, kk, ns, sb, psum, sq, locals(), stage, dump)
                    kk.barrier()
                if stage <= 4:
                    dump("omixA", omix[:, :, 0:512], [128, NT, 512], B_om, BF16)
                    continue
                with ExitStack() as ds:
                    build_diff(nc, kk, ds, sb, psum, sq, locals(), stage, dump)
                    kk.barrier()
                if stage == 5:
                    dump("omix", omix[:], [128, NT, D], B_om, BF16)
                    continue
                with ExitStack() as ts:
                    ptrO = [psum(f"ptrO{i}", [128, KC, 128], BF16, ts) for i in range(2)]
                    B_ptrO = [kk.pbuf() for _ in range(2)]
                    for i in range(NT):
                        p = i % 2
                        for c in range(KC):
                            kk.op("pe", lambda e, p=p, c=c, i=i: e.transpose(ptrO[p][:, c, :], omix[:, i, c * 128:(c + 1) * 128], ident_b[:]),
                                  reads=[B_om[i], B_const], writes=[B_ptrO[p]])
                        if i % 2 == 0:
                            kk.op("act", lambda e, p=p, i=i: e.copy(out=xT[:, :, i * 128:(i + 1) * 128], in_=ptrO[p][:]),
                                  reads=[B_ptrO[p]], writes=[B_xT[i]])
                        else:
                            kk.op("dve", lambda e, p=p, i=i: e.tensor_copy(out=xT[:, :, i * 128:(i + 1) * 128], in_=ptrO[p][:]),
                                  reads=[B_ptrO[p]], writes=[B_xT[i]])
                    kk.barrier()
                mx.close()
                with ExitStack() as ms:
                    build_tail(nc, kk, ms, sb, psum, sq, locals(), stage, dump)
                    kk.barrier()

        kk.barrier()
        E = kk.eng["sp"]
        for key, val_ in kk.out_dmas:
            if E.known.get(key, 0) < val_:
                E.h.wait_ge(kk._semof(key), val_)
                E.known[key] = val_
    return nc, dbg_out


def build_nsa(nc, kk, ns, sb, psum, sq, L, stage, dump):
    xT, omix, gates = L["xT"], L["omix"], L["gates"]
    B_xT, B_om, B_gates, B_const = L["B_xT"], L["B_om"], L["B_gates"], L["B_const"]
    win_d, hcd = L["win_d"], L["hcd"]
    ident_f, mask4, ovl = L["ident_f"], L["mask4"], L["ovl"]
    TN, TC, B_TN, B_TC = L["TN"], L["TC"], L["B_TN"], L["B_TC"]
    w2k, w2v, B_cw = L["w2k"], L["w2v"], L["B_cw"]
    ck1_d, cv1_d, posk_d, posv_d = L["ck1_d"], L["cv1_d"], L["posk_d"], L["posv_d"]

    qa = [sb(f"qa{g}", [96, NT, 4, 128], BF16, ns) for g in range(2)]
    ksa = [sb(f"ksa{g}", [96, S], BF16, ns) for g in range(2)]
    kwT = [sb(f"kwT{g}", [64, S], BF16, ns) for g in range(2)]
    vsa = [sb(f"vsa{g}", [128, NT, 65], BF16, ns) for g in range(2)]
    vwa = [sb(f"vwa{g}", [128, NT, 65], BF16, ns) for g in range(2)]
    kcmp = [sb(f"kcmp{g}", [64, 127], BF16, ns) for g in range(2)]
    vcmp = [sb(f"vcmp{g}", [127, 64], BF16, ns) for g in range(2)]
    imp = [sb(f"imp{g}", [128, NT, 32], F32, ns) for g in range(2)]
    B_kcmp = [kk.buf() for _ in range(2)]
    B_vcmp = [kk.buf() for _ in range(2)]
    B_imp = [kk.buf() for _ in range(2)]
    B_qa = [[kk.buf(f"qa{g}_{i}") for i in range(NT)] for g in range(2)]
    B_qm = [[kk.buf(f"qm{g}_{i}") for i in range(NT)] for g in range(2)]
    B_ks = [kk.buf() for g in range(2)]
    B_kw = [kk.buf() for g in range(2)]
    B_kc = [kk.buf() for g in range(2)]
    B_vc = [kk.buf() for g in range(2)]
    B_vs = [kk.buf() for g in range(2)]
    B_vw = [kk.buf() for g in range(2)]
    B_ee = kk.buf()
    for g in range(2):
        kk.dma("pool", ksa[g][64:96, :], hcd["eexp"].ap(), writes=[B_ee])
        kk.op("pool", lambda e, g=g: e.memset(vsa[g][:, :, 64:65], 1.0), writes=[B_vs[g]])
        kk.op("pool", lambda e, g=g: e.memset(vwa[g][:, :, 64:65], 1.0), writes=[B_vw[g]])
    mid = ns.enter_context(ExitStack())
    kvc = [sb(f"kvc{g}", [128, S], BF16, mid) for g in range(2)]
    ts0 = mid.enter_context(ExitStack())
    wA, B_wA = L["W1"], L["B_W1"]

    if True:
        ts = ts0
        pp = [psum(f"ppA{i}", [128, 512], F32, ts) for i in range(4)]
        B_pp = [kk.pbuf() for _ in range(4)]
        cnt = [0]

        def fm_pair(col0, dst_fns, dst_bufs_fns):
            for tg in range(4):
                p = cnt[0] % 4
                cnt[0] += 1
                for c in range(KC):
                    kk.op("pe", lambda e, p=p, c=c, tg=tg: e.matmul(
                        pp[p][:, :], lhsT=wA[:, c, col0:col0 + 128], rhs=xT[:, c, tg * 512:(tg + 1) * 512],
                        start=(c == 0), stop=(c == KC - 1)),
                        reads=[B_wA] + B_xT[tg * 4:tg * 4 + 4], writes=[B_pp[p]])
                for half in range(2):
                    dst = dst_fns[half](tg)
                    src = pp[p][64 * half:64 * half + 64, :].rearrange("p (a b) -> p a b", a=4)
                    same = (dst.base_partition() == 64 * half)
                    if same and half == 0:
                        kk.op("act", lambda e, dst=dst, src=src: e.copy(out=dst, in_=src),
                              reads=[B_pp[p]], writes=dst_bufs_fns[half](tg))
                    else:
                        kk.op("dve", lambda e, dst=dst, src=src: e.tensor_copy(out=dst, in_=src),
                              reads=[B_pp[p]], writes=dst_bufs_fns[half](tg))

        for g in range(2):
            for rp in range(2):
                hh = 4 * g + 2 * rp
                fm_pair(C_QA + hh * 64,
                        [lambda tg, g=g, r=2 * rp: qa[g][0:64, tg * 4:tg * 4 + 4, r, :],
                         lambda tg, g=g, r=2 * rp + 1: qa[g][0:64, tg * 4:tg * 4 + 4, r, :]],
                        [lambda tg, g=g: B_qa[g][tg * 4:tg * 4 + 4], lambda tg, g=g: B_qa[g][tg * 4:tg * 4 + 4]])
        for (c0, dst, bb, r0) in ((C_KC, kvc, B_kc, 0), (C_VC, kvc, B_vc, 64), (C_KS, ksa, B_ks, 0), (C_KW, kwT, B_kw, 0)):
            fm_pair(c0,
                    [lambda tg, dst=dst, r0=r0, g=g_: dst[g][r0:r0 + 64, tg * 512:(tg + 1) * 512].rearrange("p (a b) -> p a b", a=4)
                     for g_ in range(2)],
                    [lambda tg, bb=bb, g=g_: [bb[g]] for g_ in range(2)])
        for i in range(NT):
            p = cnt[0] % 4
            cnt[0] += 1
            for c in range(KC):
                kk.op("pe", lambda e, p=p, c=c, i=i: e.matmul(
                    pp[p][:, 0:128], lhsT=xT[:, c, i * 128:(i + 1) * 128], rhs=wA[:, c, C_VS:C_VS + 128],
                    start=(c == 0), stop=(c == KC - 1)), reads=[B_wA, B_xT[i]], writes=[B_pp[p]])
            for c in range(KC):
                kk.op("pe", lambda e, p=p, c=c, i=i: e.matmul(
                    pp[p][:, 128:280], lhsT=xT[:, c, i * 128:(i + 1) * 128], rhs=wA[:, c, C_VW:C_VW + 152],
                    start=(c == 0), stop=(c == KC - 1)), reads=[B_wA, B_xT[i]], writes=[B_pp[p]])
            for g in range(2):
                kk.op("dve", lambda e, p=p, g=g, i=i: e.tensor_copy(out=vsa[g][:, i, 0:64], in_=pp[p][:, g * 64:(g + 1) * 64]),
                      reads=[B_pp[p]], writes=[B_vs[g]])
                kk.op("dve", lambda e, p=p, g=g, i=i: e.tensor_copy(out=vwa[g][:, i, 0:64], in_=pp[p][:, 128 + g * 64:128 + (g + 1) * 64]),
                      reads=[B_pp[p]], writes=[B_vw[g]])
            kk.op("act", lambda e, p=p, i=i: e.activation(out=gates[:, i, :], in_=pp[p][:, 256:280], func=AF.Sigmoid),
                  reads=[B_pp[p]], writes=[B_gates])
        L["load_W1"]("B")
        kk.barrier()
        ts0.close()
    if stage == 2:
        dump("qa0", qa[0][0:64], [64, NT, 4, 128], B_qa[0], BF16)
        dump("ks0", ksa[0][:], [96, S], [B_ks[0], B_ee], BF16)
        dump("vs1", vsa[1][:], [128, NT, 65], [B_vs[1]], BF16)
        dump("gates", gates[:], [128, NT, 24], [B_gates])
        return

    with ExitStack() as ts:
        w1 = sb("w1", [128, 32, 128], BF16, ts)
        posT = sb("posT", [128, 32], BF16, ts)
        cpos = sb("cpos", [128, 2], F32, ts)
        B_w1 = kk.buf()
        B_cpos = kk.buf()
        kk.dma("pool", w1[0:64], ck1_d.ap().rearrange("(j d) h -> d j h", d=64), writes=[B_w1])
        kk.dma("pool", w1[64:128], cv1_d.ap().rearrange("(j d) h -> d j h", d=64), writes=[B_w1])
        with nc.allow_non_contiguous_dma(reason="tiny pos transpose"):
            kk.dma("pool", posT[0:64, :], posk_d.ap().rearrange("j d -> d j"), writes=[B_w1])
            kk.dma("pool", posT[64:128, :], posv_d.ap().rearrange("j d -> d j"), writes=[B_w1])
        pc = [psum(f"pcm{i}", [128, 512], F32, ts) for i in range(2)]
        B_pc = [kk.pbuf() for _ in range(2)]
        zz = [sb(f"zz{i}", [128, 4, 127], F32, ts) for i in range(2)]
        gl = [sb(f"gl{i}", [128, 127], BF16, ts) for i in range(2)]
        B_zz = [kk.buf() for _ in range(2)]
        B_gl = [kk.buf() for _ in range(2)]
        for kv in range(2):
            r0 = 64 * kv
            for j in range(32):
                kk.op("pe", lambda e, kv=kv, r0=r0, j=j: e.matmul(
                    pc[0][:, 500 + kv:501 + kv], lhsT=w1[r0:r0 + 64, j, :], rhs=posT[r0:r0 + 64, j:j + 1],
                    start=(j == 0), stop=(j == 31)), reads=[B_w1], writes=[B_pc[0]])
        kk.op("dve", lambda e: e.tensor_copy(out=cpos[:], in_=pc[0][:, 500:502]), reads=[B_pc[0]], writes=[B_cpos])
        it = 0
        for g in range(2):
            for kv, (bsrc, w2) in enumerate(((B_kc, w2k), (B_vc, w2v))):
                p = it % 2
                it += 1
                r0 = 64 * kv
                for j in range(32):
                    kk.op("pe", lambda e, p=p, j=j, g=g, r0=r0: e.matmul(
                        pc[p][:, 0:127], lhsT=w1[r0:r0 + 64, j, :], rhs=kvc[g][r0:r0 + 64, j:j + 16 * 126 + 1:16],
                        start=(j == 0), stop=(j == 31)), reads=[B_w1, bsrc[g]], writes=[B_pc[p]])
                z = zz[p]
                kk.op("dve", lambda e, p=p, kv=kv, z=z: e.tensor_scalar_add(out=z[:, 0, :], in0=pc[p][:, 0:127],
                                                                          scalar1=cpos[:, kv:kv + 1]),
                      reads=[B_pc[p], B_cpos], writes=[B_zz[p]])
                kk.op("dve", lambda e, z=z: e.tensor_tensor(out=z[:, 1, :], in0=z[:, 0, :], in1=z[:, 0, :], op=ALU.mult),
                      reads=[B_zz[p]], writes=[B_zz[p]])
                kk.op("dve", lambda e, z=z: e.tensor_scalar(out=z[:, 1, :], in0=z[:, 1, :], scalar1=0.044715, scalar2=1.0,
                                                            op0=ALU.mult, op1=ALU.add), reads=[B_zz[p]], writes=[B_zz[p]])
                kk.op("dve", lambda e, z=z: e.tensor_tensor(out=z[:, 2, :], in0=z[:, 1, :], in1=z[:, 0, :], op=ALU.mult),
                      reads=[B_zz[p]], writes=[B_zz[p]])
                kk.op("act", lambda e, z=z: e.activation(out=z[:, 3, :], in_=z[:, 2, :], func=AF.Sigmoid,
                                                         scale=2.0 * math.sqrt(2.0 / math.pi)),
                      reads=[B_zz[p]], writes=[B_zz[p]])
                kk.op("dve", lambda e, z=z, p=p: e.tensor_tensor(out=gl[p][:], in0=z[:, 3, :], in1=z[:, 0, :], op=ALU.mult),
                      reads=[B_zz[p]], writes=[B_gl[p]])
                if kv == 0:
                    kk.op("pe", lambda e, p=p, w2=w2: e.matmul(pc[p][0:64, 128:255], lhsT=w2[:], rhs=gl[p][:],
                                                              start=True, stop=True),
                          reads=[B_cw, B_gl[p]], writes=[B_pc[p]])
                    kk.op("dve", lambda e, p=p, g=g: e.tensor_copy(out=kcmp[g][:], in_=pc[p][0:64, 128:255]),
                          reads=[B_pc[p]], writes=[B_kcmp[g]])
                else:
                    kk.op("pe", lambda e, p=p, w2=w2: e.matmul(pc[p][0:127, 256:320], lhsT=gl[p][:], rhs=w2[:],
                                                              start=True, stop=True),
                          reads=[B_cw, B_gl[p]], writes=[B_pc[p]])
                    kk.op("dve", lambda e, p=p, g=g: e.tensor_copy(out=vcmp[g][:], in_=pc[p][0:127, 256:320]),
                          reads=[B_pc[p]], writes=[B_vcmp[g]])
        kk.barrier()
    mid.close()
    if stage == 3:
        dump("kcmp0", kcmp[0][:], [64, 127], [B_kcmp[0]], BF16)
        dump("vcmp1", vcmp[1][:], [127, 64], [B_vcmp[1]], BF16)
        return

    with ExitStack() as ts:
        psc = [psum(f"psc{i}", [128, 4, 128], F32, ts) for i in range(2)]
        ptp = [psum(f"ptp{i}", [128, 4, 128], F32, ts) for i in range(2)]
        ptb = [psum(f"ptb{i}", [128, 8, 128], BF16, ts) for i in range(2)]
        pov = [psum(f"pov{i}", [128, 512], F32, ts) for i in range(2)]
        B_ptb = [kk.pbuf() for _ in range(2)]
        B_psc = [kk.pbuf() for _ in range(2)]
        B_ptp = [kk.pbuf() for _ in range(2)]
        B_pov = [kk.pbuf() for _ in range(2)]
        s_sb = [sb(f"s_sb{i}", [128, 4, 127], F32, ts) for i in range(2)]
        e_sb = [sb(f"e_sb{i}", [128, 4, 127], F32, ts) for i in range(2)]
        pT = [sb(f"pT{i}", [127, 4, 128], BF16, ts) for i in range(2)]
        pbf = [sb(f"pbf{i}", [128, 4, 127], BF16, ts) for i in range(2)]
        psm = [sb(f"psm{i}", [128, 128], F32, ts) for i in range(2)]
        B_pbf = [kk.buf() for _ in range(2)]
        B_psm = [kk.buf() for _ in range(2)]
        for i_ in range(2):
            kk.op("pool", lambda e, i_=i_: e.memset(psm[i_][:], 0.0), writes=[B_psm[i_]])
        stc = [sb(f"stc{i}", [128, 16], F32, ts) for i in range(2)]
        B_s = [kk.buf() for _ in range(2)]
        B_e = [kk.buf() for _ in range(2)]
        B_pT = [kk.buf() for _ in range(2)]
        B_stc = [kk.buf() for _ in range(2)]
        its = [(g, tb) for g in range(2) for tb in range(NT)]

        def cmp_p1a(k):
            g, tb = its[k]
            p = k % 2
            for r in range(4):
                kk.op("pe", lambda e, p=p, g=g, tb=tb, r=r: e.matmul(
                    psc[p][:, r, 0:127], lhsT=qa[g][0:64, tb, r, :], rhs=kcmp[g][:], start=True, stop=True),
                    reads=[B_qa[g][tb], B_kcmp[g]], writes=[B_psc[p]])
            off = 120 - 8 * tb
            kk.op("dve", lambda e, p=p, g=g, off=off: e.tensor_tensor(
                out=s_sb[p][:], in0=psc[p][:, :, 0:127], in1=TC[:, g, :, off:off + 127], op=ALU.add),
                reads=[B_psc[p], B_TC], writes=[B_s[p]])
            for r in range(4):
                kk.op("act", lambda e, p=p, r=r: e.activation(out=e_sb[p][:, r, :], in_=s_sb[p][:, r, :], func=AF.Exp,
                                                             scale=SC_A, accum_out=stc[p][:, r:r + 1]),
                      reads=[B_s[p]], writes=[B_e[p], B_stc[p]])

        def cmp_p1b(k):
            g, tb = its[k]
            p = k % 2
            kk.op("dve", lambda e, p=p: e.tensor_scalar_max(out=stc[p][:, 4:8], in0=stc[p][:, 0:4], scalar1=TINY),
                  reads=[B_stc[p]], writes=[B_stc[p]])
            kk.op("dve", lambda e, p=p: e.reciprocal(out=stc[p][:, 8:12], in_=stc[p][:, 4:8]),
                  reads=[B_stc[p]], writes=[B_stc[p]])
            kk.op("pool", lambda e, p=p: e.tensor_tensor(
                out=pbf[p][:], in0=e_sb[p][:], in1=stc[p][:, 8:12].unsqueeze(2).to_broadcast([128, 4, 127]), op=ALU.mult),
                reads=[B_e[p], B_stc[p]], writes=[B_pbf[p]])
            kk.op("dve", lambda e, p=p: e.tensor_scalar_mul(out=psm[p][:, 0:127], in0=e_sb[p][:, 0, :], scalar1=stc[p][:, 8:9]),
                  reads=[B_e[p], B_stc[p]], writes=[B_psm[p]])
            for r in range(1, 4):
                kk.op("dve", lambda e, p=p, r=r: e.scalar_tensor_tensor(
                    out=psm[p][:, 0:127], in0=e_sb[p][:, r, :], scalar=stc[p][:, 8 + r:9 + r], in1=psm[p][:, 0:127],
                    op0=ALU.mult, op1=ALU.add), reads=[B_e[p], B_stc[p], B_psm[p]], writes=[B_psm[p]])
            kk.op("dve", lambda e, p=p, g=g, tb=tb: e.reduce_sum(out=imp[g][:, tb, :], in_=psm[p][:].rearrange("p (j i) -> p j i", i=4),
                                                             axis=AX.X), reads=[B_psm[p]], writes=[B_imp[g]])
            kk.op("dve", lambda e, p=p, g=g, tb=tb: e.tensor_tensor(out=imp[g][:, tb, 1:32], in0=imp[g][:, tb, 1:32],
                                                                in1=psm[p][:, 3:127:4], op=ALU.add),
                  reads=[B_psm[p], B_imp[g]], writes=[B_imp[g]])

        def cmp_p2(k):
            p = k % 2
            for r in range(4):
                kk.op("pe", lambda e, p=p, r=r: e.transpose(ptb[p][0:127, r, :], pbf[p][:, r, :], L["ident_b"][:]),
                      reads=[B_pbf[p], B_const], writes=[B_ptb[p]])
            kk.op("act", lambda e, p=p: e.copy(out=pT[p][:], in_=ptb[p][0:127, 0:4, :]), reads=[B_ptb[p]], writes=[B_pT[p]])

        def cmp_p3(k):
            g, tb = its[k]
            p = k % 2
            for r in range(4):
                kk.op("pe", lambda e, p=p, r=r, g=g: e.matmul(pov[p][:, r * 64:(r + 1) * 64], lhsT=pT[p][:, r, :],
                                                              rhs=vcmp[g][:], start=True, stop=True),
                      reads=[B_pT[p], B_vcmp[g]], writes=[B_pov[p]])
            for r in range(4):
                col = g * 12 + r * 3
                kk.op("act", lambda e, p=p, g=g, tb=tb, r=r, col=col: e.activation(
                    out=omix[:, tb, g * 256 + r * 64:g * 256 + (r + 1) * 64], in_=pov[p][:, r * 64:(r + 1) * 64],
                    func=AF.Copy, scale=gates[:, tb, col:col + 1]),
                    reads=[B_pov[p], B_gates], writes=[B_om[tb]])

        n_it = len(its)
        for k in range(n_it + 3):
            if k < n_it:
                cmp_p1a(k)
            if 0 <= k - 1 < n_it:
                cmp_p1b(k - 1)
            if 0 <= k - 2 < n_it:
                cmp_p2(k - 2)
            if 0 <= k - 3 < n_it:
                cmp_p3(k - 3)
        nfv = sb("nfv", [128, 16, 32], F32, ts)
        addc = sb("addc", [128, 16, 32], F32, ts)
        validt = sb("validt", [128, 16, 32], F32, ts)
        kk.dma("sp", nfv[:], hcd["nfv"].ap(), writes=[B_const])
        kk.dma("sp", addc[:], hcd["addc"].ap(), writes=[B_const])
        kk.dma("sp", validt[:], hcd["valid"].ap(), writes=[B_const])
        sc_t = sb("sc_t", [128, NT, 32], F32, ts)
        m8 = sb("m8", [128, NT, 8], F32, ts)
        stg = [sb(f"stg{i}", [128, 96], F32, ts) for i in range(2)]
        B_sc = kk.buf()
        B_m8 = kk.buf()
        B_stg = [kk.buf() for _ in range(2)]
        for i_ in range(2):
            kk.op("pool", lambda e, i_=i_: e.memset(stg[i_][:], 0.0), writes=[B_stg[i_]])
        for g in range(2):
            kk.op("dve", lambda e, g=g: e.tensor_tensor(out=sc_t[:], in0=imp[g][:], in1=nfv[:], op=ALU.mult),
                  reads=[B_imp[g], B_const], writes=[B_sc])
            kk.op("dve", lambda e: e.tensor_tensor(out=sc_t[:], in0=sc_t[:], in1=addc[:], op=ALU.add),
                  reads=[B_sc, B_const], writes=[B_sc])
            for tb in range(NT):
                kk.op("dve", lambda e, tb=tb: e.max(out=m8[:, tb, :], in_=sc_t[:, tb, :]), reads=[B_sc], writes=[B_m8])
            kk.op("dve", lambda e: e.tensor_tensor(out=sc_t[:], in0=sc_t[:], in1=m8[:, :, 7:8].to_broadcast([128, NT, 32]),
                                                   op=ALU.is_ge), reads=[B_sc, B_m8], writes=[B_sc])
            kk.op("dve", lambda e: e.tensor_tensor(out=sc_t[:], in0=sc_t[:], in1=validt[:], op=ALU.mult),
                  reads=[B_sc, B_const], writes=[B_sc])
            kk.op("dve", lambda e: e.tensor_scalar(out=sc_t[:], in0=sc_t[:], scalar1=-1.0, scalar2=-NEGB,
                                                   op0=ALU.add, op1=ALU.mult), reads=[B_sc], writes=[B_sc])
            for tb in range(NT):
                p = tb % 2
                kk.op("dve", lambda e, p=p, tb=tb: e.tensor_copy(out=stg[p][:, 64:96], in_=sc_t[:, tb, :]),
                      reads=[B_sc], writes=[B_stg[p]])
                kk.op("pe", lambda e, p=p, tb=tb: e.transpose(ptp[p][0:96, 0, :], stg[p][:], ident_f[:]),
                      reads=[B_stg[p], B_const], writes=[B_ptp[p]])
                kk.op("act", lambda e, p=p, g=g, tb=tb: e.copy(
                    out=qa[g][64:96, tb, :, :], in_=ptp[p][64:96, 0:1, :].to_broadcast([32, 4, 128])),
                    reads=[B_ptp[p]], writes=[B_qm[g][tb]])
        kk.barrier()
    if stage == 4 and False:
        return

    with ExitStack() as ts:
        NSB = 6
        pss = [psum(f"pss{i}", [128, 512], F32, ts) for i in range(NSB)]
        po_t = [psum(f"po{i}", [128, 512], F32, ts) for i in range(2)]
        po = [t_[:, 0:260].rearrange("p (c d) -> p c d", d=65) for t_ in po_t]
        B_pss = [kk.pbuf() for _ in range(NSB)]
        B_po = [kk.pbuf() for _ in range(4)]
        NPT = 8
        pt = [sb(f"ptS{i}", [128, 512], BF16, ts) for i in range(NPT)]
        B_pt = [kk.buf() for _ in range(NPT)]
        fac = [sb(f"fac{i}", [128, 16], F32, ts) for i in range(2)]
        B_fac = [kk.buf() for _ in range(2)]
        ocs = [sb(f"ocs{i}", [128, 2, 4, 65], F32, ts) for i in range(2)]
        B_ocs = [[kk.buf() for _ in range(2)] for _ in range(2)]
        oacc = [sb(f"oacc{i}", [128, 4, 64], F32, ts) for i in range(2)]
        B_oacc = [kk.buf() for _ in range(2)]
        tiles = []
        it = 0
        for g in range(2):
            for tb in range(NT):
                par = it % 2
                it += 1
                for br in range(2):
                    kbs = list(range(0, tb + 1)) if br == 0 else list(range(max(0, tb - 4), tb + 1))
                    for kb in kbs:
                        tiles.append((g, tb, par, br, kb, kbs))

        def sw_S(idx):
            g, tb, par, br, kb, kbs = tiles[idx]
            p = idx % NSB
            q = idx % NPT
            if br == 0:
                kk.op("pe", lambda e: e.matmul(
                    pss[p][:, :], lhsT=ksa[g][0:96, kb * 128:(kb + 1) * 128],
                    rhs=qa[g][0:96, tb, :, :].rearrange("p r t -> p (r t)"), start=True, stop=True),
                    reads=[B_ks[g], B_ee, B_qa[g][tb], B_qm[g][tb]], writes=[B_pss[p]])
            else:
                kk.op("pe", lambda e: e.matmul(
                    pss[p][:, :], lhsT=kwT[g][0:64, kb * 128:(kb + 1) * 128],
                    rhs=qa[g][0:64, tb, :, :].rearrange("p r t -> p (r t)"), start=True, stop=True),
                    reads=[B_kw[g], B_qa[g][tb]], writes=[B_pss[p]])
            if kb == tb or kb == tb - 1:
                seg = 0 if kb == tb else 1
                kk.op("dve", lambda e: e.tensor_tensor(
                    out=pss[p][:, :], in0=pss[p][:, :], in1=TN[:, g, seg, :, :].rearrange("p r t -> p (r t)"),
                    op=ALU.add), reads=[B_pss[p], B_TN], writes=[B_pss[p]])
            elif br == 1 and kb == tb - 4:
                kk.op("dve", lambda e: e.tensor_tensor(
                    out=pss[p][:, :].rearrange("p (r t) -> p r t", r=4),
                    in0=pss[p][:, :].rearrange("p (r t) -> p r t", r=4),
                    in1=mask4[:].unsqueeze(1).to_broadcast([128, 4, 128]), op=ALU.add),
                    reads=[B_pss[p], B_const], writes=[B_pss[p]])
            kk.op("act", lambda e: e.activation(out=pt[q][:], in_=pss[p][:, :], func=AF.Exp, scale=SC_A),
                  reads=[B_pss[p]], writes=[B_pt[q]])

        def sw_PV(idx):
            g, tb, par, br, kb, kbs = tiles[idx]
            q = idx % NPT
            pob = po[br]
            B_pob = B_po[br]
            va = vsa if br == 0 else vwa
            Bv = B_vs if br == 0 else B_vw
            if kb == kbs[0]:
                kk.drain(until_tag=("po", br))
            for r in range(4):
                kk.op("pe", lambda e, r=r: e.matmul(
                    pob[:, r, :], lhsT=pt[q][:, r * 128:(r + 1) * 128], rhs=va[g][:, kb, :],
                    start=(kb == kbs[0] and r == 0), stop=(kb == kbs[-1]), skip_group_check=True),
                    reads=[B_pt[q], Bv[g]], writes=[B_pob])
            if kb != kbs[-1]:
                return
            f = fac[par]
            bidx = 1 if br == 0 else 2
            tag = ("po", br)
            oc_ = ocs[par][:, br]
            B_oc_ = B_ocs[par][br]
            kk.defer("dve", lambda e: e.tensor_copy(out=oc_, in_=pob), reads=[B_pob], writes=[B_oc_], tag=tag)
            kk.defer("dve", lambda e: e.tensor_scalar_max(
                out=f[:, br * 8:br * 8 + 4].unsqueeze(2), in0=oc_[:, :, 64:65], scalar1=TINY),
                reads=[B_oc_], writes=[B_fac[par]])
            kk.defer("dve", lambda e: e.reciprocal(out=f[:, br * 8 + 4:br * 8 + 8], in_=f[:, br * 8:br * 8 + 4]),
                     reads=[B_fac[par]], writes=[B_fac[par]])
            kk.defer("dve", lambda e: e.tensor_tensor(
                out=f[:, br * 8 + 4:br * 8 + 8].unsqueeze(2), in0=f[:, br * 8 + 4:br * 8 + 8].unsqueeze(2),
                in1=gates[:, tb, g * 12:(g + 1) * 12].rearrange("p (r b) -> p r b", b=3)[:, :, bidx:bidx + 1],
                op=ALU.mult), reads=[B_fac[par], B_gates], writes=[B_fac[par]])
            if br == 0:
                kk.defer("dve", lambda e: e.tensor_tensor(
                    out=oacc[par][:], in0=oc_[:, :, 0:64], in1=f[:, 4:8].unsqueeze(2).to_broadcast([128, 4, 64]),
                    op=ALU.mult), reads=[B_oc_, B_fac[par]], writes=[B_oacc[par]])
                kk.defer("dve", lambda e: e.tensor_tensor(
                    out=oacc[par][:], in0=oacc[par][:],
                    in1=omix[:, tb, g * 256:(g + 1) * 256].rearrange("p (r d) -> p r d", r=4), op=ALU.add),
                    reads=[B_oacc[par], B_om[tb]], writes=[B_oacc[par]])
            else:
                kk.defer("dve", lambda e: e.tensor_tensor(
                    out=oc_[:, :, 0:64], in0=oc_[:, :, 0:64], in1=f[:, 12:16].unsqueeze(2).to_broadcast([128, 4, 64]),
                    op=ALU.mult), reads=[B_oc_, B_fac[par]], writes=[B_oc_])
                kk.defer("dve", lambda e: e.tensor_tensor(
                    out=omix[:, tb, g * 256:(g + 1) * 256].rearrange("p (r d) -> p r d", r=4),
                    in0=oc_[:, :, 0:64], in1=oacc[par][:], op=ALU.add),
                    reads=[B_oc_, B_oacc[par]], writes=[B_om[tb]])

        LA = 5
        for idx in range(len(tiles) + LA):
            if idx < len(tiles):
                sw_S(idx)
            if idx - LA >= 0:
                sw_PV(idx - LA)
            kk.drain(2)
        kk.drain()
        kk.barrier()


def build_diff(nc, kk, ds, sb, psum, sq, L, stage, dump):
    xT, omix = L["xT"], L["omix"]
    B_xT, B_om, B_const = L["B_xT"], L["B_om"], L["B_const"]
    win_d = L["win_d"]
    TD, B_TD, neglam, B_l, subln = L["TD"], L["B_TD"], L["neglam"], L["B_l"], L["subln"]

    qb = sb("qbT", [128, 4, S], BF16, ds)
    kb_ = sb("kbT", [128, 4, S], BF16, ds)
    vb = sb("vb", [128, NT, 8, 65], BF16, ds)
    B_qb = [[kk.buf() for _ in range(4)] for _ in range(4)]
    B_kb = [kk.buf() for _ in range(4)]
    B_vb = kk.buf()
    kk.op("pool", lambda e: e.memset(vb[:, :, :, 64:65], 1.0), writes=[B_vb])
    NCOL = 1536
    wv = win_d.ap().rearrange("(kc p) c -> p kc c", p=128)

    with ExitStack() as ts:
        wB, B_wB = L["W1"], L["B_W1"]
        pp = [psum(f"ppB{i}", [128, 512], F32, ts) for i in range(4)]
        B_pp = [kk.pbuf() for _ in range(4)]
        cnt = 0
        for which, dst in ((0, qb), (1, kb_)):
            for ch in range(4):
                col0 = which * 512 + ch * 128
                for tg in range(4):
                    p = cnt % 4
                    cnt += 1
                    for c in range(KC):
                        kk.op("pe", lambda e, p=p, c=c, tg=tg, col0=col0: e.matmul(
                            pp[p][:, :], lhsT=wB[:, c, col0:col0 + 128], rhs=xT[:, c, tg * 512:(tg + 1) * 512],
                            start=(c == 0), stop=(c == KC - 1)),
                            reads=[B_wB] + B_xT[tg * 4:tg * 4 + 4], writes=[B_pp[p]])
                    bw = [B_qb[ch][tg]] if which == 0 else [B_kb[ch]]
                    if cnt % 2 == 0:
                        kk.op("act", lambda e, p=p, dst=dst, ch=ch, tg=tg: e.copy(out=dst[:, ch, tg * 512:(tg + 1) * 512], in_=pp[p][:, :]),
                              reads=[B_pp[p]], writes=bw)
                    else:
                        kk.op("dve", lambda e, p=p, dst=dst, ch=ch, tg=tg: e.tensor_copy(out=dst[:, ch, tg * 512:(tg + 1) * 512], in_=pp[p][:, :]),
                              reads=[B_pp[p]], writes=bw)
        for i in range(NT):
            p = cnt % 4
            cnt += 1
            for c in range(KC):
                kk.op("pe", lambda e, p=p, c=c, i=i: e.matmul(
                    pp[p][:, :], lhsT=xT[:, c, i * 128:(i + 1) * 128], rhs=wB[:, c, 1024:1536],
                    start=(c == 0), stop=(c == KC - 1)), reads=[B_wB, B_xT[i]], writes=[B_pp[p]])
            kk.op("dve", lambda e, p=p, i=i: e.tensor_copy(out=vb[:, i, :, 0:64], in_=pp[p][:, :].rearrange("p (h d) -> p h d", h=8)),
                  reads=[B_pp[p]], writes=[B_vb])
        L["load_W1"]("O")
        kk.barrier()

    with ExitStack() as ts:
        NSB = 4
        NS2 = 3
        pss2 = [psum(f"psd{i}", [128, 2, 512], F32, ts) for i in range(NS2)]
        po_t = [psum(f"pod{i}", [128, 512], F32, ts) for i in range(2)]
        po = [t_[:, 0:260].rearrange("p (c d) -> p c d", d=65) for t_ in po_t]
        B_pss = [kk.pbuf() for _ in range(NSB)]
        B_po = [kk.pbuf() for _ in range(4)]
        NPT = 6
        NPT = 4
        pt = [sb(f"ptD{i}", [128, 2, 512], BF16, ts) for i in range(NPT)]
        B_pt = [kk.buf() for _ in range(NPT)]
        fac = [sb(f"facD{i}", [128, 24], F32, ts) for i in range(2)]
        B_fac = [kk.buf() for _ in range(2)]
        oc = [sb(f"ocD{i}", [128, 2, 4, 65], F32, ts) for i in range(2)]
        B_oc = [kk.buf() for _ in range(2)]
        o1 = [sb(f"o1D{i}", [128, 4, 64], F32, ts) for i in range(2)]
        o2 = [sb(f"o2D{i}", [128, 4, 64], F32, ts) for i in range(2)]
        B_o1 = [kk.buf() for _ in range(2)]
        B_o2 = [kk.buf() for _ in range(2)]
        tiles = []
        it = 0
        for h in range(8):
            for G in range(4):
                par = it % 2
                it += 1
                for kb in range(4 * G + 4):
                    tiles.append((h, G, par, kb))

        def d_S(idx):
            h, G, par, kb = tiles[idx]
            ch = h // 2
            j = max(0, kb - 4 * G)
            n0 = 128 * j
            N = 512 - n0
            p = idx % NS2
            q = idx % NPT
            for mp in range(2):
                base = (h % 2) * 64 + mp * 32
                kw = {"tile_position": (base, 0)} if base == 96 else {}
                kk.op("pe", lambda e, mp=mp, base=base, kw=kw: e.matmul(
                    pss2[p][:, mp, 0:N], lhsT=kb_[base:base + 32, ch, kb * 128:(kb + 1) * 128],
                    rhs=qb[base:base + 32, ch, G * 512 + n0:(G + 1) * 512], start=True, stop=True, **kw),
                    reads=[B_kb[ch], B_qb[ch][G]], writes=[B_pss[p]])
            if kb >= 4 * G:
                w = min(256, N)
                kk.op("dve", lambda e: e.tensor_tensor(
                    out=pss2[p][:, :, 0:w], in0=pss2[p][:, :, 0:w], in1=TD[:, h, 0:w].unsqueeze(1).to_broadcast([128, 2, w]),
                    op=ALU.add), reads=[B_pss[p], B_TD], writes=[B_pss[p]])
            elif kb == 4 * G - 1:
                kk.op("dve", lambda e: e.tensor_tensor(
                    out=pss2[p][:, :, 0:128], in0=pss2[p][:, :, 0:128],
                    in1=TD[:, h, 128:256].unsqueeze(1).to_broadcast([128, 2, 128]), op=ALU.add),
                    reads=[B_pss[p], B_TD], writes=[B_pss[p]])
            kk.op("act", lambda e: e.activation(out=pt[q][:, :, 0:N], in_=pss2[p][:, :, 0:N], func=AF.Exp, scale=SC_B),
                  reads=[B_pss[p]], writes=[B_pt[q]])

        def d_PV(idx):
            h, G, par, kb = tiles[idx]
            nkb = 4 * G + 4
            j = max(0, kb - 4 * G)
            q = idx % NPT
            for mp in range(2):
                pob = po[mp]
                B_pob = B_po[mp]
                if kb == 0:
                    kk.drain(until_tag=("pod", mp))
                for c in range(j, 4):
                    kbl = 4 * G + c
                    kk.op("pe", lambda e, c=c, kbl=kbl, mp=mp, pob=pob: e.matmul(
                        pob[:, c, :], lhsT=pt[q][:, mp, (c - j) * 128:(c - j + 1) * 128], rhs=vb[:, kb, h, :],
                        start=(kb == 0 and c == 0), stop=(kb == kbl), skip_group_check=True),
                        reads=[B_pt[q], B_vb], writes=[B_pob])
            if kb != nkb - 1:
                return

            f = fac[par]
            p0, p1 = oc[par][:, 0], oc[par][:, 1]
            Bp0 = Bp1 = B_oc[par]
            kk.defer("dve", lambda e: e.tensor_copy(out=oc[par][:, 0], in_=po[0]),
                     reads=[B_po[0]], writes=[B_oc[par]], tag=("pod", 0))
            kk.defer("dve", lambda e: e.tensor_copy(out=oc[par][:, 1], in_=po[1]),
                     reads=[B_po[1]], writes=[B_oc[par]], tag=("pod", 1))
            kk.defer("dve", lambda e: e.tensor_scalar_max(out=f[:, 0:8].rearrange("p (m c) -> p m c", m=2).unsqueeze(3),
                                                          in0=oc[par][:, :, :, 64:65], scalar1=TINY),
                     reads=[B_oc[par]], writes=[B_fac[par]])
            kk.defer("dve", lambda e: e.reciprocal(out=f[:, 8:16], in_=f[:, 0:8]), reads=[B_fac[par]], writes=[B_fac[par]])
            kk.defer("dve", lambda e: e.tensor_scalar_mul(out=f[:, 12:16], in0=f[:, 12:16], scalar1=neglam[:, 0:1]),
                     reads=[B_fac[par], B_l], writes=[B_fac[par]])
            kk.defer("dve", lambda e: e.tensor_tensor(
                out=o1[par][:], in0=p0[:, :, 0:64], in1=f[:, 8:12].unsqueeze(2).to_broadcast([128, 4, 64]), op=ALU.mult),
                reads=[Bp0, B_fac[par]], writes=[B_o1[par]])
            kk.defer("dve", lambda e: e.tensor_tensor(
                out=o2[par][:], in0=p1[:, :, 0:64], in1=f[:, 12:16].unsqueeze(2).to_broadcast([128, 4, 64]), op=ALU.mult),
                reads=[Bp1, B_fac[par]], writes=[B_o2[par]])
            kk.defer("dve", lambda e: e.tensor_tensor(out=o1[par][:], in0=o1[par][:], in1=o2[par][:], op=ALU.add),
                     reads=[B_o1[par], B_o2[par]], writes=[B_o1[par]])
            kk.defer("dve", lambda e: e.tensor_tensor(out=o2[par][:], in0=o1[par][:], in1=o1[par][:], op=ALU.mult),
                     reads=[B_o1[par]], writes=[B_o2[par]])
            kk.defer("dve", lambda e: e.reduce_sum(out=f[:, 16:20], in_=o2[par][:], axis=AX.X),
                     reads=[B_o2[par]], writes=[B_fac[par]])
            kk.defer("dve", lambda e: e.tensor_scalar(out=f[:, 20:24], in0=f[:, 16:20], scalar1=1.0 / 64, scalar2=EPS,
                                                      op0=ALU.mult, op1=ALU.add), reads=[B_fac[par]], writes=[B_fac[par]])
            kk.defer("act", lambda e: e.activation(out=f[:, 20:24], in_=f[:, 20:24], func=AF.Ln),
                     reads=[B_fac[par]], writes=[B_fac[par]])
            kk.defer("act", lambda e: e.activation(out=f[:, 16:20], in_=f[:, 20:24], func=AF.Exp, scale=-0.5),
                     reads=[B_fac[par]], writes=[B_fac[par]])
            kk.defer("dve", lambda e: e.tensor_tensor(
                out=o1[par][:], in0=o1[par][:], in1=f[:, 16:20].unsqueeze(2).to_broadcast([128, 4, 64]), op=ALU.mult),
                reads=[B_o1[par], B_fac[par]], writes=[B_o1[par]])
            kk.defer("dve", lambda e: e.scalar_tensor_tensor(
                out=omix[:, 4 * G:4 * G + 4, 512 + h * 64:512 + (h + 1) * 64], in0=o1[par][:], scalar=1.0 - LAMBDA_INIT,
                in1=subln[:].unsqueeze(1).to_broadcast([128, 4, 64]), op0=ALU.mult, op1=ALU.mult),
                reads=[B_o1[par], B_const], writes=B_om[4 * G:4 * G + 4])

        LA = 2
        for idx in range(len(tiles) + LA):
            if idx < len(tiles):
                d_S(idx)
            if idx - LA >= 0:
                d_PV(idx - LA)
            kk.drain(2)
        kk.drain()
        kk.barrier()


def build_tail(nc, kk, ms, sb, psum, sq, L, stage, dump):
    xT, omix = L["xT"], L["omix"]
    B_xT, B_om, B_const = L["B_xT"], L["B_om"], L["B_const"]
    x_d, out_d, wout_d, wg_d, wu_d, wd_d = L["x_d"], L["out_d"], L["wout_d"], L["wg_d"], L["wu_d"], L["wd_d"]
    ident_f, ident_b, wr, rbias = L["ident_f"], L["ident_b"], L["wr"], L["rbias"]
    lnffn_d, lnfin_d, bcast_rows = L["lnffn_d"], L["lnfin_d"], L["bcast_rows"]

    hres = sb("hres", [128, NT, D], F32, ms)
    B_h = [kk.buf(f"h{i}") for i in range(NT)]
    comb = sb("comb", [128, NT, 32], F32, ms)
    B_comb = kk.buf("comb")

    NWB = 2
    wgu = [sb("wgu0", [128, 2, KC, 512], BF16, ms)]
    wdn = [sb("wdn0", [128, 2, 2, D], BF16, ms)]
    B_wgu = [kk.buf() for _ in range(NWB)]
    B_wdn = [kk.buf() for _ in range(NWB)]

    def load_pair(pr, nobarrier=False):
        w = pr % NWB
        for ei in range(2):
            e_ = 2 * pr + ei
            kk.dma("pool", wgu[w][:, ei, :, 0:256], wg_d.ap()[e_].rearrange("(kc p) f -> p kc f", p=128), writes=[B_wgu[w]], nobarrier=nobarrier)
            kk.dma("pool", wgu[w][:, ei, :, 256:512], wu_d.ap()[e_].rearrange("(kc p) f -> p kc f", p=128), writes=[B_wgu[w]], nobarrier=nobarrier)
            kk.dma("pool", wdn[w][:, ei, :, :], wd_d.ap()[e_].rearrange("(fc p) d -> p fc d", p=128), writes=[B_wdn[w]], nobarrier=nobarrier)

    if stage > 7:
        load_pair(0, nobarrier=True)

    with ExitStack() as ts:
        wo, B_wo = L["W1"], L["B_W1"]
        py = [psum(f"pyO{i}", [128, D], F32, ts) for i in range(2)]
        B_py = [kk.pbuf() for _ in range(2)]
        for i in range(NT):
            kk.dma("sp", hres[:, i, :], x_d.ap()[sq, i * 128:(i + 1) * 128, :], writes=[B_h[i]])
        for i in range(NT):
            p = i % 2
            for hf in range(2):
                for c in range(KC):
                    kk.op("pe", lambda e, p=p, c=c, i=i, hf=hf: e.matmul(
                        py[p][:, hf * 512:(hf + 1) * 512], lhsT=xT[:, c, i * 128:(i + 1) * 128],
                        rhs=wo[:, c, hf * 512:(hf + 1) * 512], start=(c == 0), stop=(c == KC - 1)),
                        reads=[B_xT[i], B_wo], writes=[B_py[p]])
            kk.op("dve", lambda e, p=p, i=i: e.tensor_tensor(out=hres[:, i, :], in0=hres[:, i, :], in1=py[p][:], op=ALU.add),
                  reads=[B_py[p], B_h[i]], writes=[B_h[i]])
        if sq + 1 < L["nseq"]:
            L["load_W1"]("A")
        kk.barrier()
    if stage == 6:
        dump("h1", hres[:], [128, NT, D], B_h)
        return

    lg = sb("lg", [128, NT, 36], F32, ms)
    B_lg = kk.buf()
    with ExitStack() as ts:
        gffn = sb("gffn", [128, D], F32, ts)
        B_gf = kk.buf()
        kk.dma("sp", gffn[:], bcast_rows(lnffn_d, D), writes=[B_gf])
        NB1 = 2
        tn = [sb(f"tn{i}", [128, D], F32, ts) for i in range(NB1)]
        tTf = [sb(f"tTf{i}", [128, KC, 128], F32, ts) for i in range(2)]
        st = sb("stM", [128, NT, 4], F32, ts)
        ptr = [psum(f"ptrM{i}", [128, KC, 128], F32, ts) for i in range(2)]
        plg = [psum(f"plg{i}", [128, 512], F32, ts) for i in range(2)]
        B_tn = [kk.buf() for _ in range(NB1)]
        B_tTf = [kk.buf() for _ in range(2)]
        B_ptr = [kk.pbuf() for _ in range(2)]
        B_plg = [kk.pbuf() for _ in range(2)]
        B_st = [kk.buf() for _ in range(NT)]
        B_junk = kk.buf()

        B_stall = kk.buf()
        for i in range(NT):
            jb, Bj = tn[i % NB1], B_tn[i % NB1]
            kk.op("act", lambda e, i=i, jb=jb: e.activation(out=jb[:], in_=hres[:, i, :], func=AF.Square,
                                                            accum_out=st[:, i, 0:1]),
                  reads=[B_h[i]], writes=[B_stall, Bj])
        kk.op("act", lambda e: e.activation(out=st[:, :, 1:2], in_=st[:, :, 0:1], func=AF.Sqrt, bias=EPS, scale=1.0 / D),
              reads=[B_stall], writes=[B_stall])
        kk.op("dve", lambda e: e.reciprocal(out=st[:, :, 2:3], in_=st[:, :, 1:2]), reads=[B_stall], writes=[B_stall])

        def r_s1(i):
            p = i % NB1
            kk.op("dve", lambda e: e.scalar_tensor_tensor(
                out=tn[p][:], in0=hres[:, i, :], scalar=st[:, i, 2:3], in1=gffn[:], op0=ALU.mult, op1=ALU.mult),
                reads=[B_h[i], B_stall, B_gf], writes=[B_tn[p]])

        def r_s2(i):
            p = i % NB1
            q = i % 2
            for c in range(KC):
                kk.op("pe", lambda e, c=c: e.transpose(ptr[q][:, c, :], tn[p][:, c * 128:(c + 1) * 128], ident_f[:]),
                      reads=[B_tn[p], B_const], writes=[B_ptr[q]])
            kk.op("act", lambda e: e.copy(out=tTf[q][:], in_=ptr[q][:]), reads=[B_ptr[q]], writes=[B_tTf[q]])
            kk.op("pool", lambda e: e.tensor_copy(out=xT[:, :, i * 128:(i + 1) * 128], in_=tTf[q][:]),
                  reads=[B_tTf[q]], writes=[B_xT[i]])
            for c in range(KC):
                kk.op("pe", lambda e, c=c: e.matmul(plg[q][:, 0:36], lhsT=tTf[q][:, c, :], rhs=wr[:, c, :],
                                                    start=(c == 0), stop=(c == KC - 1)),
                      reads=[B_tTf[q], B_const], writes=[B_plg[q]])
            kk.op("dve", lambda e: e.tensor_tensor(out=lg[:, i, :], in0=plg[q][:, 0:36], in1=rbias[:], op=ALU.add),
                  reads=[B_plg[q], B_const], writes=[B_lg])

        for k in range(NT + 1):
            if k < NT:
                r_s1(k)
            if k >= 1:
                r_s2(k - 1)
        rt = sb("rt", [128, NT, 42], F32, ts)
        r4 = sb("r4", [128, NT, 8], F32, ts)
        m8 = sb("m8M", [128, NT, 8], F32, ts)
        B_rt = kk.buf()
        lgg = lg[:, :, 0:4]
        lge = lg[:, :, 4:36].rearrange("p t (g e) -> p t g e", g=4)
        mg, sg, gp, ohg = rt[:, :, 0:1], rt[:, :, 1:2], rt[:, :, 2:3], rt[:, :, 4:8]
        eg = rt[:, :, 8:12]
        ein, ex, selm = rt[:, :, 16:24], rt[:, :, 24:32], rt[:, :, 32:40]
        den, fc_ = rt[:, :, 40:41], rt[:, :, 41:42]
        R = dict(reads=[B_lg, B_rt], writes=[B_rt])
        kk.op("dve", lambda e: e.tensor_reduce(out=mg, in_=lgg, axis=AX.X, op=ALU.max), **R)
        kk.op("dve", lambda e: e.tensor_tensor(out=eg, in0=lgg, in1=mg.to_broadcast([128, NT, 4]), op=ALU.subtract), **R)
        kk.op("act", lambda e: e.activation(out=eg, in_=eg, func=AF.Exp), **R)
        kk.op("dve", lambda e: e.reduce_sum(out=sg, in_=eg, axis=AX.X), **R)
        kk.op("dve", lambda e: e.reciprocal(out=gp, in_=sg), **R)
        kk.op("dve", lambda e: e.tensor_tensor(out=ohg, in0=lgg, in1=mg.to_broadcast([128, NT, 4]), op=ALU.is_equal), **R)
        kk.op("dve", lambda e: e.tensor_tensor(out=ein, in0=lge[:, :, 0, :], in1=ohg[:, :, 0:1].to_broadcast([128, NT, 8]), op=ALU.mult), **R)
        for g_ in range(1, 4):
            kk.op("dve", lambda e, g_=g_: e.tensor_tensor(out=r4[:], in0=lge[:, :, g_, :],
                                                        in1=ohg[:, :, g_:g_ + 1].to_broadcast([128, NT, 8]), op=ALU.mult), **R)
            kk.op("dve", lambda e: e.tensor_tensor(out=ein, in0=ein, in1=r4[:], op=ALU.add), **R)
        for tb in range(NT):
            kk.op("dve", lambda e, tb=tb: e.max(out=m8[:, tb, :], in_=rt[:, tb, 16:24]), **R)
        kk.op("dve", lambda e: e.tensor_tensor(out=ex, in0=ein, in1=m8[:, :, 0:1].to_broadcast([128, NT, 8]), op=ALU.subtract), **R)
        kk.op("act", lambda e: e.activation(out=ex, in_=ex, func=AF.Exp), **R)
        kk.op("dve", lambda e: e.tensor_tensor(out=selm, in0=ein, in1=m8[:, :, 1:2].to_broadcast([128, NT, 8]), op=ALU.is_ge), **R)
        kk.op("dve", lambda e: e.tensor_tensor(out=ex, in0=ex, in1=selm, op=ALU.mult), **R)
        kk.op("dve", lambda e: e.reduce_sum(out=den, in_=ex, axis=AX.X), **R)
        kk.op("dve", lambda e: e.reciprocal(out=fc_, in_=den), **R)
        kk.op("dve", lambda e: e.tensor_tensor(out=fc_, in0=fc_, in1=gp, op=ALU.mult), **R)
        kk.op("dve", lambda e: e.tensor_tensor(out=ex, in0=ex, in1=fc_.to_broadcast([128, NT, 8]), op=ALU.mult), **R)
        kk.op("dve", lambda e: e.tensor_tensor(
            out=comb[:].rearrange("p t (g e) -> p t g e", g=4), in0=ohg.unsqueeze(3).to_broadcast([128, NT, 4, 8]),
            in1=ex.unsqueeze(2).to_broadcast([128, NT, 4, 8]), op=ALU.mult), reads=[B_rt], writes=[B_comb])
        kk.barrier()
    if stage == 7:
        dump("comb", comb[:], [128, NT, 32], [B_comb])
        dump("tT", xT[:], [128, KC, S], B_xT, BF16)
        return

    with ExitStack() as ts:
        wgu.append(sb("wgu1", [128, 2, KC, 512], BF16, ts))
        wdn.append(sb("wdn1", [128, 2, 2, D], BF16, ts))
        pgu = [psum(f"pgu{i}", [128, 512], F32, ts) for i in range(2)]
        ptrE = [psum(f"ptrE{i}", [128, 8, 128], BF16, ts) for i in range(2)]
        py = [psum(f"pyE{i}", [128, D], F32, ts) for i in range(2)]
        B_pgu = [kk.pbuf() for _ in range(2)]
        B_ptr = [kk.pbuf() for _ in range(2)]
        B_py = [kk.pbuf() for _ in range(2)]
        sgs = [sb(f"sgs{i}", [128, 256], F32, ts) for i in range(3)]
        hh = [sb(f"hh{i}", [128, 256], BF16, ts) for i in range(3)]
        hT = [sb(f"hT{i}", [128, 2, 128], BF16, ts) for i in range(4)]
        B_sgs = [kk.buf() for _ in range(3)]
        B_hh = [kk.buf() for _ in range(3)]
        B_hT = [kk.buf() for _ in range(4)]

        NPAIR = L['nexp'] // 2
        units = [(pr, i, ei) for pr in range(NPAIR) for i in range(NT) for ei in range(2)]
        if NPAIR > 1:
            load_pair(1)

        def m_A(k):
            pr, i, ei = units[k]
            w = pr % NWB
            e_ = 2 * pr + ei
            p = k % 3
            pb = k % 2
            for c in range(KC):
                kk.op("pe", lambda e, c=c: e.matmul(
                    pgu[pb][:, :], lhsT=xT[:, c, i * 128:(i + 1) * 128], rhs=wgu[w][:, ei, c, :],
                    start=(c == 0), stop=(c == KC - 1)), reads=[B_xT[i], B_wgu[w]], writes=[B_pgu[pb]])
            kk.op("act", lambda e: e.activation(out=sgs[p][:], in_=pgu[pb][:, 0:256], func=AF.Silu),
                  reads=[B_pgu[pb]], writes=[B_sgs[p]])
            kk.op("dve", lambda e: e.scalar_tensor_tensor(
                out=hh[p][:], in0=pgu[pb][:, 256:512], scalar=comb[:, i, e_:e_ + 1], in1=sgs[p][:],
                op0=ALU.mult, op1=ALU.mult), reads=[B_pgu[pb], B_sgs[p], B_comb], writes=[B_hh[p]])

        def m_B(k):
            p = k % 3
            s_ = k % 4
            tb_ = k % 2
            for fc in range(2):
                kk.op("pe", lambda e, fc=fc: e.transpose(ptrE[tb_][:, fc, :], hh[p][:, fc * 128:(fc + 1) * 128], ident_b[:]),
                      reads=[B_hh[p], B_const], writes=[B_ptr[tb_]])
            kk.op("act", lambda e: e.copy(out=hT[s_][:], in_=ptrE[tb_][:, 0:2, :]),
                  reads=[B_ptr[tb_]], writes=[B_hT[s_]])

        def m_C(k):
            pr, i, ei = units[k]
            w = pr % NWB
            yp = (k // 2) % 2
            slots = [(k - 1) % 4, k % 4]
            for e2 in range(2):
                for fc in range(2):
                    for hf in range(2):
                        kk.op("pe", lambda e, e2=e2, fc=fc, hf=hf: e.matmul(
                            py[yp][:, hf * 512:(hf + 1) * 512], lhsT=hT[slots[e2]][:, fc, :],
                            rhs=wdn[w][:, e2, fc, hf * 512:(hf + 1) * 512],
                            start=(e2 == 0 and fc == 0), stop=(e2 == 1 and fc == 1)),
                            reads=[B_hT[slots[e2]], B_wdn[w]], writes=[B_py[yp]])
            kk.op("dve", lambda e: e.tensor_tensor(out=hres[:, i, :], in0=hres[:, i, :], in1=py[yp][:], op=ALU.add),
                  reads=[B_py[yp], B_h[i]], writes=[B_h[i]])

        nu = len(units)
        for k in range(nu + 2):
            if k < nu:
                m_A(k)
            if 0 <= k - 1 < nu:
                m_B(k - 1)
            if 0 <= k - 2 < nu and units[k - 2][2] == 1:
                m_C(k - 2)
                pr_, i_, _ = units[k - 2]
                if i_ == NT - 1 and pr_ + 2 < NPAIR:
                    load_pair(pr_ + 2)
        kk.barrier()

    with ExitStack() as ts:
        gfin = sb("gfin", [128, D], F32, ts)
        B_gfi = kk.buf()
        kk.dma("sp", gfin[:], bcast_rows(lnfin_d, D), writes=[B_gfi])
        NOB = 3
        ob = [sb(f"ob{i}", [128, D], F32, ts) for i in range(NOB)]
        junk = [sb(f"junkF{i}", [128, D], BF16, ts) for i in range(2)]
        st = sb("stF", [128, 3, NT], F32, ts)
        B_ob = [kk.buf() for _ in range(NOB)]
        B_st = kk.buf()
        B_junk = [kk.buf() for _ in range(2)]
        B_sth = [kk.buf() for _ in range(2)]
        HN = NT // 2
        for hf in range(2):
            lo, hi = hf * HN, (hf + 1) * HN
            for i in range(lo, hi):
                kk.op("act", lambda e, i=i: e.activation(out=junk[i % 2][:], in_=hres[:, i, :], func=AF.Square,
                                                        accum_out=st[:, 0, i:i + 1]),
                      reads=[B_h[i]], writes=[B_sth[hf], B_junk[i % 2]])
            kk.op("act", lambda e, lo=lo, hi=hi: e.activation(out=st[:, 1, lo:hi], in_=st[:, 0, lo:hi], func=AF.Sqrt,
                                                           bias=EPS, scale=1.0 / D),
                  reads=[B_sth[hf]], writes=[B_sth[hf]])
            kk.op("dve", lambda e, lo=lo, hi=hi: e.reciprocal(out=st[:, 2, lo:hi], in_=st[:, 1, lo:hi]),
                  reads=[B_sth[hf]], writes=[B_sth[hf]])
        for i in range(NT):
            p = i % NOB
            hf = i // HN
            kk.op("dve", lambda e, p=p, i=i: e.scalar_tensor_tensor(
                out=ob[p][:], in0=hres[:, i, :], scalar=st[:, 2, i:i + 1], in1=gfin[:], op0=ALU.mult, op1=ALU.mult),
                reads=[B_h[i], B_sth[hf], B_gfi], writes=[B_ob[p]])
            kk.dma("sp", out_d.ap()[sq, i * 128:(i + 1) * 128, :], ob[p][:], reads=[B_ob[p]], is_output=True)
        kk.barrier()


_CACHE = {}


def _prep_inputs(inputs, core, nseq):
    m = {}
    m["x"] = np.ascontiguousarray(inputs["x"][core * nseq:(core + 1) * nseq])
    m["rel_bias"] = np.ascontiguousarray(inputs["rel_bias"])
    for k_ in ("ln_mix", "w_in", "cmp_pos_k", "cmp_pos_v", "cmp_k_w1", "cmp_k_w2", "cmp_v_w1", "cmp_v_w2",
               "diff_lq1", "diff_lk1", "diff_lq2", "diff_lk2", "diff_subln", "w_out", "ln_ffn",
               "router_group_w", "router_group_b", "router_expert_w", "router_expert_b",
               "exp_w_gate", "exp_w_up", "exp_w_down"):
        a = np.asarray(inputs[k_])
        if k_ in ("ln_mix", "diff_lq1", "diff_lk1", "diff_lq2", "diff_lk2", "diff_subln", "ln_ffn",
                  "router_group_b", "router_expert_b"):
            m[k_] = np.ascontiguousarray(a.reshape(1, -1))
        else:
            m[k_] = np.ascontiguousarray(a[0])
    m["ln_final"] = np.ascontiguousarray(np.asarray(inputs["ln_final"]).reshape(1, -1))
    return m


def kernel(**inputs):
    inputs = {k_: np.asarray(v_) for k_, v_ in inputs.items()}
    n_cores = 8
    nseq = inputs["x"].shape[0] // n_cores
    if "nc" not in _CACHE:
        _CACHE["nc"] = build_nc(nseq=nseq)[0]
        _CACHE["hc"] = _host_consts()
    nc = _CACHE["nc"]
    hc = _CACHE["hc"]
    shared = _prep_inputs(inputs, 0, nseq)
    in_maps = []
    for c in range(n_cores):
        m = dict(shared)
        m["x"] = np.ascontiguousarray(inputs["x"][c * nseq:(c + 1) * nseq])
        for k_, v_ in hc.items():
            m["hc_" + k_] = v_
        in_maps.append(m)
    res = run_bass_kernel_spmd(nc, in_maps, core_ids=list(range(n_cores)))
    out = np.concatenate([np.asarray(r["out"]) for r in res.results], axis=0)
    return out.astype(np.float32)
```
